# Optimizing a Trainium2 kernel written in Bass

```python
import math
import jax, jax.numpy as jnp
from jax import lax
import numpy as np

D_MODEL = 2048
BATCH = 4
SEQ = 4096
DEPTH = 4

EPS = 1e-5
POOL_WINDOWS = (2, 4, 8, 16)
POOL_GROUPS = 4
POOL_GROUP_DIM = D_MODEL // 16
POOL_DIM = POOL_GROUPS * POOL_GROUP_DIM
CONV_DIM = D_MODEL // 4
CONV_KERNEL = 31
ATTN_HEAD_DIM = 128
ATTN_HEADS = D_MODEL // 256
ATTN_DIM = ATTN_HEADS * ATTN_HEAD_DIM
Q_BLOCK = 128
GMLP_GROUPS = 4
GMLP_GROUP_DIM = D_MODEL // 16
GMLP_DIM = GMLP_GROUPS * GMLP_GROUP_DIM
GMLP_CHUNK = 128
IN_SIZES = (POOL_DIM, 2 * CONV_DIM, ATTN_DIM, ATTN_DIM, ATTN_DIM, ATTN_HEADS, 2 * GMLP_DIM)
IN_DIM = sum(IN_SIZES)
BRANCH_DIMS = (POOL_DIM, CONV_DIM, ATTN_DIM, GMLP_DIM)
N_BRANCHES = 4
MIX_DIM = sum(BRANCH_DIMS)
N_EXPERTS = 32
TOP_K = 4
D_FF_EXPERT = 3 * D_MODEL // 8
SWIGLU_LIMIT = 7.0
SWIGLU_ALPHA = 1.702
MOE_BLOCK = 128

kernel_name = "hybrid_pool_conv_fox_gmlp_moe_adaln"


def _split_points(sizes):
    pts, acc = [], 0
    for s in sizes[:-1]:
        acc += s
        pts.append(acc)
    return pts


def rms_norm(x, g):
    xf = x.astype(jnp.float32)
    y = xf * lax.rsqrt(jnp.mean(xf * xf, axis=-1, keepdims=True) + EPS)
    return (y * g.astype(jnp.float32)).astype(x.dtype)


def layer_norm(x, g, b):
    xf = x.astype(jnp.float32)
    mu = jnp.mean(xf, axis=-1, keepdims=True)
    xc = xf - mu
    y = xc * lax.rsqrt(jnp.mean(xc * xc, axis=-1, keepdims=True) + EPS)
    return (y * g.astype(jnp.float32) + b.astype(jnp.float32)).astype(x.dtype)


def modulate(h, shift, scale):
    return h * (1 + scale[:, None, :]) + shift[:, None, :]


def pool_mixer(a, pool_w, pool_scale):
    B, S, _ = a.shape
    af = a.reshape(B, S, POOL_GROUPS, POOL_GROUP_DIM).astype(jnp.float32)
    cs = jnp.pad(jnp.cumsum(af, axis=1), ((0, 0), (1, 0), (0, 0), (0, 0)))
    t = jnp.arange(S)
    pooled = []
    for g, w in enumerate(POOL_WINDOWS):
        csg = cs[:, :, g]
        upper = csg[:, 1:]
        lower = jnp.pad(csg[:, :S + 1 - w], ((0, 0), (w - 1, 0), (0, 0)))
        count = jnp.minimum(t + 1, w).astype(jnp.float32)[None, :, None]
        pooled.append((upper - lower) / count)
    mixed = (jnp.stack(pooled, axis=2) - af).astype(a.dtype)
    y = jnp.einsum('bsgc,gcd->bsgd', mixed, pool_w)
    return y.reshape(B, S, POOL_DIM) * pool_scale


def conv_mixer(z, conv_w, conv_b, ln_g, ln_b):
    za, zb = jnp.split(z, 2, axis=-1)
    glu = za * jax.nn.sigmoid(zb)
    y = lax.conv_general_dilated(glu, conv_w[:, None, :], window_strides=(1,),
                                 padding=[(CONV_KERNEL - 1, 0)],
                                 dimension_numbers=('NWC', 'WIO', 'NWC'),
                                 feature_group_count=CONV_DIM) + conv_b
    return jax.nn.silu(layer_norm(y, ln_g, ln_b))


def forgetting_attention(q, k, v, f_logit):
    B, S, H, Dh = q.shape
    F = jnp.cumsum(jax.nn.log_sigmoid(f_logit.astype(jnp.float32)), axis=1)
    nb = S // Q_BLOCK
    qb = q.reshape(B, nb, Q_BLOCK, H, Dh).transpose(1, 0, 2, 3, 4)
    Fq = F.reshape(B, nb, Q_BLOCK, H).transpose(1, 0, 2, 3)
    Fk = F.transpose(0, 2, 1)
    kpos = jnp.arange(S)
    scale = 1.0 / math.sqrt(Dh)

    def block(args):
        i, qi, Fqi = args
        s = jnp.einsum('bqhd,bkhd->bhqk', qi, k).astype(jnp.float32) * scale
        s = s + Fqi.transpose(0, 2, 1)[..., None] - Fk[:, :, None, :]
        qpos = i * Q_BLOCK + jnp.arange(Q_BLOCK)
        s = jnp.where(kpos[None, :] <= qpos[:, None], s, -jnp.inf)
        p = jax.nn.softmax(s, axis=-1)
        return jnp.einsum('bhqk,bkhd->bqhd', p.astype(v.dtype), v)

    out = lax.map(block, (jnp.arange(nb), qb, Fq))
    return out.transpose(1, 0, 2, 3, 4).reshape(B, S, H * Dh)


def gmlp_mixer(z, ln_g, ws, bs):
    z = jax.nn.gelu(z)
    u, v = jnp.split(z, 2, axis=-1)
    v = rms_norm(v, ln_g)
    B, S, _ = v.shape
    n = S // GMLP_CHUNK
    vc = v.reshape(B, n, GMLP_CHUNK, GMLP_GROUPS, GMLP_GROUP_DIM)
    causal = jnp.tril(jnp.ones((GMLP_CHUNK, GMLP_CHUNK), dtype=bool))
    wm = jnp.where(causal[None], ws, 0).astype(v.dtype)
    sv = jnp.einsum('gts,bnsgc->bntgc', wm, vc) + bs.T[None, None, :, :, None]
    return u * sv.reshape(B, S, GMLP_DIM)


def moe_ffn(h, router_w, router_b, w_up, b_up, w_down, b_down):
    B, S, D = h.shape
    T = B * S
    hf = h.reshape(T, D)
    logits = (hf @ router_w).astype(jnp.float32) + router_b.astype(jnp.float32)
    top_val, top_idx = lax.top_k(logits, TOP_K)
    gates = jax.nn.softmax(top_val, axis=-1)
    A = T * TOP_K
    flat_e = top_idx.reshape(A)
    flat_tok = jnp.arange(A, dtype=jnp.int32) // TOP_K
    flat_gate = gates.reshape(A)
    order = jnp.argsort(flat_e)
    sorted_e = flat_e[order]
    counts = jnp.bincount(flat_e, length=N_EXPERTS)
    padded = (counts + MOE_BLOCK - 1) // MOE_BLOCK * MOE_BLOCK
    start = jnp.cumsum(counts) - counts
    pend = jnp.cumsum(padded)
    pstart = pend - padded
    dest = pstart[sorted_e] + jnp.arange(A) - start[sorted_e]
    n_blocks = -(-(A + N_EXPERTS * (MOE_BLOCK - 1)) // MOE_BLOCK)
    NP = n_blocks * MOE_BLOCK
    tok_buf = jnp.full((NP,), T, dtype=jnp.int32).at[dest].set(flat_tok[order])
    gate_buf = jnp.zeros((NP,), jnp.float32).at[dest].set(flat_gate[order])
    block_e = jnp.minimum(jnp.searchsorted(pend, jnp.arange(n_blocks) * MOE_BLOCK, side='right'),
                          N_EXPERTS - 1)
    h_pad = jnp.concatenate([hf, jnp.zeros((1, D), hf.dtype)], axis=0)

    def expert_block(args):
        toks, e = args
        xb = h_pad[toks]
        gu = xb @ w_up[e] + b_up[e]
        g, lin = jnp.split(gu, 2, axis=-1)
        g = jnp.minimum(g, SWIGLU_LIMIT)
        lin = jnp.clip(lin, -SWIGLU_LIMIT, SWIGLU_LIMIT)
        act = g * jax.nn.sigmoid(SWIGLU_ALPHA * g) * (lin + 1)
        return act @ w_down[e] + b_down[e]

    out = lax.map(expert_block, (tok_buf.reshape(n_blocks, MOE_BLOCK), block_e))
    out = out.reshape(NP, D) * gate_buf[:, None].astype(out.dtype)
    y = jnp.zeros((T + 1, D), out.dtype).at[tok_buf].add(out)[:T]
    return y.reshape(B, S, D)


def setup_inputs(seed: int = 0) -> dict:
    key = jax.random.key(seed)
    ks = jax.random.split(key, 32)
    L, D, E, F = DEPTH, D_MODEL, N_EXPERTS, D_FF_EXPERT

    def nrm(k, shape, s):
        return jax.random.normal(k, shape, jnp.float32) * s

    return {
        'x': nrm(ks[0], (BATCH, SEQ, D), 1.0),
        'c': nrm(ks[1], (BATCH, D), 1.0),
        'w_ada': nrm(ks[2], (L, D, 6 * D), 0.2 * D ** -0.5),
        'b_ada': nrm(ks[3], (L, 6 * D), 0.02),
        'g_norm_mix': 1.0 + nrm(ks[4], (L, D), 0.02),
        'w_in': nrm(ks[5], (L, D, IN_DIM), D ** -0.5),
        'pool_w': nrm(ks[6], (L, POOL_GROUPS, POOL_GROUP_DIM, POOL_GROUP_DIM), POOL_GROUP_DIM ** -0.5),
        'pool_scale': 1.0 + nrm(ks[7], (L, POOL_DIM), 0.02),
        'conv_w': nrm(ks[8], (L, CONV_KERNEL, CONV_DIM), CONV_KERNEL ** -0.5),
        'conv_b': nrm(ks[9], (L, CONV_DIM), 0.02),
        'conv_ln_g': 1.0 + nrm(ks[10], (L, CONV_DIM), 0.02),
        'conv_ln_b': nrm(ks[11], (L, CONV_DIM), 0.02),
        'fgate_b': jax.random.uniform(ks[12], (L, ATTN_HEADS), jnp.float32, 1.0, 6.0),
        'gmlp_ln_g': 1.0 + nrm(ks[13], (L, GMLP_DIM), 0.02),
        'gmlp_ws': nrm(ks[14], (L, GMLP_GROUPS, GMLP_CHUNK, GMLP_CHUNK), 0.05),
        'gmlp_bs': 1.0 + nrm(ks[15], (L, GMLP_GROUPS, GMLP_CHUNK), 0.02),
        'w_gate': nrm(ks[16], (L, D, N_BRANCHES * D), D ** -0.5),
        'b_gate': nrm(ks[17], (L, N_BRANCHES * D), 0.02),
        'w_branch': nrm(ks[18], (L, MIX_DIM, D), (MIX_DIM / N_BRANCHES) ** -0.5),
        'w_o': nrm(ks[19], (L, D, D), D ** -0.5),
        'g_norm_ffn': 1.0 + nrm(ks[20], (L, D), 0.02),
        'router_w': nrm(ks[21], (L, D, E), D ** -0.5),
        'router_b': nrm(ks[22], (L, E), 0.01),
        'w_up': nrm(ks[23], (L, E, D, 2 * F), D ** -0.5),
        'b_up': nrm(ks[24], (L, E, 2 * F), 0.02),
        'w_down': nrm(ks[25], (L, E, F, D), F ** -0.5),
        'b_down': nrm(ks[26], (L, E, D), 0.02),
        'g_final': 1.0 + nrm(ks[27], (D,), 0.02),
    }


def reference(x, c, w_ada, b_ada, g_norm_mix, w_in, pool_w, pool_scale, conv_w, conv_b, conv_ln_g,
              conv_ln_b, fgate_b, gmlp_ln_g, gmlp_ws, gmlp_bs, w_gate, b_gate, w_branch, w_o,
              g_norm_ffn, router_w, router_b, w_up, b_up, w_down, b_down, g_final):
    B, S, D = x.shape
    in_pts = _split_points(IN_SIZES)
    br_pts = [0] + _split_points(BRANCH_DIMS) + [MIX_DIM]
    c_act = jax.nn.silu(c)
    for l in range(DEPTH):
        mod = c_act @ w_ada[l] + b_ada[l]
        sh1, sc1, gt1, sh2, sc2, gt2 = jnp.split(mod, 6, axis=-1)
        h = modulate(rms_norm(x, g_norm_mix[l]), sh1, sc1)
        z = h @ w_in[l]
        z_pool, z_conv, q, k, v, f_logit, z_gmlp = jnp.split(z, in_pts, axis=-1)
        y_a = pool_mixer(z_pool, pool_w[l], pool_scale[l])
        y_b = conv_mixer(z_conv, conv_w[l], conv_b[l], conv_ln_g[l], conv_ln_b[l])
        y_c = forgetting_attention(q.reshape(B, S, ATTN_HEADS, ATTN_HEAD_DIM),
                                   k.reshape(B, S, ATTN_HEADS, ATTN_HEAD_DIM),
                                   v.reshape(B, S, ATTN_HEADS, ATTN_HEAD_DIM),
                                   f_logit + fgate_b[l])
        y_d = gmlp_mixer(z_gmlp, gmlp_ln_g[l], gmlp_ws[l], gmlp_bs[l])
        merged = jnp.zeros_like(x)
        for i, y in enumerate((y_a, y_b, y_c, y_d)):
            g = jax.nn.sigmoid(h @ w_gate[l][:, i * D:(i + 1) * D] + b_gate[l][i * D:(i + 1) * D])
            merged = merged + g * (y @ w_branch[l][br_pts[i]:br_pts[i + 1]])
        x = x + gt1[:, None, :] * (merged @ w_o[l])
        h2 = modulate(rms_norm(x, g_norm_ffn[l]), sh2, sc2)
        x = x + gt2[:, None, :] * moe_ffn(h2, router_w[l], router_b[l], w_up[l], b_up[l], w_down[l], b_down[l])
    return rms_norm(x, g_final)
```

```python
from contextlib import ExitStack
import numpy as np
import concourse.bass as bass
import concourse.mybir as mybir
from concourse.bass_utils import run_bass_kernel_spmd

F32 = mybir.dt.float32
BF16 = mybir.dt.bfloat16
AF = mybir.ActivationFunctionType
ALU = mybir.AluOpType
AX = mybir.AxisListType

D = 2048
KC = 16
SEQ = 4096
DEPTH = 4
NE = 32
FF = 768
IN_DIM = 5640
EPS = 1e-5
O_POOL, O_CA, O_CB, O_Q, O_K, O_V, O_F, O_GU, O_GV = 0, 512, 1024, 1536, 2560, 3584, 4608, 4616, 5128


class Res:
    __slots__ = ("w", "r", "dsem", "dcnt", "name")

    def __init__(self, name=""):
        self.w = None
        self.r = {}
        self.dsem = None
        self.dcnt = 0
        self.name = name


class Eng:
    def __init__(self, h, sem, is_pe=False):
        self.h = h
        self.sem = sem
        self.n = 0
        self.known = {}
        self.is_pe = is_pe


class K:
    def __init__(self, nc, es):
        self.nc = nc
        self.es = es
        sem = lambda n: es.enter_context(nc.semaphore(n))
        self.pe = Eng(nc.tensor, sem("q_pe"), True)
        self.act = Eng(nc.scalar, sem("q_act"))
        self.dve = Eng(nc.vector, sem("q_dve"))
        self.pool = Eng(nc.gpsimd, sem("q_pool"))
        self.sp = Eng(nc.sync, sem("q_sp"))
        self.engs = [self.pe, self.act, self.dve, self.pool, self.sp]
        self.dres = []
        self.nsem = 5
        self.sempool = []
        self.ps = []
        self.psr = []
        for i in range(8):
            self.ps.append(es.enter_context(nc.psum_tensor(f"psb{i}", [128, 512], F32)))
            self.psr.append(Res(f"ps{i}"))
        self.psi = 0

    def sb(self, name, shape, dt, stack=None):
        self.uid = getattr(self, "uid", 0) + 1
        t = (stack or self.es).enter_context(self.nc.sbuf_tensor(f"{name}_{self.uid}", shape, dt))
        return t, Res(name)

    def nextps(self):
        i = self.psi
        self.psi = (i + 1) % 8
        return self.ps[i], self.psr[i]

    def _need(self, q, tok):
        sem, val = tok
        if q.known.get(id(sem), 0) >= val:
            return
        q.h.wait_ge(sem, val)
        q.known[id(sem)] = val

    def _deps(self, q, R, W):
        toks = []
        for r in R:
            if r.w is not None:
                toks.append(r.w)
        for w in W:
            if w.w is not None:
                toks.append(w.w)
            toks.extend(w.r.values())
        for t in toks:
            if q.is_pe and t[0] is q.sem:
                continue
            self._need(q, t)

    def _mark(self, tok, R, W):
        for r in R:
            r.r[id(tok[0])] = tok
        for w in W:
            w.w = tok
            w.r = {}

    def op(self, q, fn, R=(), W=()):
        self._deps(q, R, W)
        ins = fn()
        q.n += 1
        ins.then_inc(q.sem, 1)
        self._mark((q.sem, q.n), R, W)
        return ins

    def mm(self, out_ap, pairs, R, W, fp32=False):
        q = self.pe
        self._deps(q, R, W)
        n = len(pairs)
        ins = None
        for i, (l, r) in enumerate(pairs):
            ins = self.nc.tensor.matmul(out_ap, lhsT=l, rhs=r, start=(i == 0), stop=(i == n - 1))
        q.n += 1
        ins.then_inc(q.sem, 1)
        self._mark((q.sem, q.n), R, W)

    def mm1(self, out_ap, l, r, start, stop, R, W):
        q = self.pe
        self._deps(q, R, W)
        ins = self.nc.tensor.matmul(out_ap, lhsT=l, rhs=r, start=start, stop=stop)
        q.n += 1
        ins.then_inc(q.sem, 1)
        self._mark((q.sem, q.n), R, W)

    def tr(self, out_ap, in_ap, ident_ap, R, W):
        q = self.pe
        self._deps(q, R, W)
        ins = self.nc.tensor.transpose(out=out_ap, in_=in_ap, identity=ident_ap)
        q.n += 1
        ins.then_inc(q.sem, 1)
        self._mark((q.sem, q.n), R, W)

    def dma(self, q, out_ap, in_ap, R, W, sres):
        if sres.dsem is None:
            if self.sempool:
                sres.dsem, sres.dcnt = self.sempool.pop()
            else:
                sres.dsem = self.es.enter_context(self.nc.semaphore(f"dsem{self.nsem}"))
                sres.dcnt = 0
                self.nsem += 1
            self.dres.append(sres)
        self._deps(q, R, W)
        ins = q.h.dma_start(out=out_ap, in_=in_ap)
        sres.dcnt += 16
        ins.then_inc(sres.dsem, 16)
        self._mark((sres.dsem, sres.dcnt), R, W)

    def barrier(self):
        toks = [(e.sem, e.n) for e in self.engs if e.n > 0]
        toks += [(r.dsem, r.dcnt) for r in self.dres if r.dcnt > 0]
        for q in self.engs:
            for t in toks:
                if t[0] is q.sem:
                    continue
                self._need(q, t)
        for r in self.dres:
            self.sempool.append((r.dsem, r.dcnt))
            r.dsem = None
        self.dres = []


def prefetcher(jobs, issue, depth):
    state = {"n": 0, "res": {}}

    def get(i):
        while state["n"] <= min(i + depth, len(jobs) - 1):
            j = state["n"]
            state["res"][j] = issue(j, jobs[j])
            state["n"] += 1
        return state["res"].pop(i)
    return get


def build(T, nlayers=DEPTH, do_moe=True, final=True, LW=DEPTH, split=False):
    NT = T // 128
    NB = T // 512
    NSB = T // 1024
    nc = bass.Bass("TRN2", target_bir_lowering=False)
    din = lambda n, s, dt=F32: nc.dram_tensor(n, s, dt, kind="ExternalInput").ap()
    x_in = din("x", [T, D])
    c_col = din("c_col", [128, KC])
    w_ada = din("w_ada", [LW, D, 6 * D])
    b_ada_col = din("b_ada_col", [LW, 128, 96])
    gmix_col = din("gmix_col", [LW, 128, KC])
    w_in_r = din("w_in_r", [LW, 11, 128, KC * 512])
    wf_r = din("wf_r", [LW, 128, KC * 8])
    pool_w = din("pool_w", [LW, 4, 128, 128])
    pscale_col = din("pscale_col", [LW, 128, 4])
    convw_col = din("convw_col", [LW, 128, 4, 31])
    convb_col = din("convb_col", [LW, 128, 4])
    clng_col = din("clng_col", [LW, 128, 4])
    clnb_col = din("clnb_col", [LW, 128, 4])
    fgb_row = din("fgb_row", [LW, 128, 8])
    glng_row = din("glng_row", [LW, 128, 512])
    gmlp_ws = din("gmlp_ws", [LW, 4, 128, 128])
    gbs_row = din("gbs_row", [LW, 128, 512])
    w_gate_r = din("w_gate_r", [LW, KC, 128, 4 * KC * 128])
    bgate_col = din("bgate_col", [LW, 128, 64])
    w_branch_r = din("w_branch_r", [LW, KC, 128, 20 * 128])
    w_o_r = din("w_o_r", [LW, KC, 128, KC * 128])
    gffn_col = din("gffn_col", [LW, 128, KC])
    router_w = din("router_w", [LW, D, NE])
    rb_row = din("rb_row", [LW, 128, 256])
    w_up_r = din("w_up_r", [LW, NE, 6, 128, KC * 256])
    bup_col = din("bup_col", [LW, 128, NE * 12])
    w_down_r = din("w_down_r", [LW, NE, 2, 128, 6 * 1024])
    b_down = din("b_down", [LW, NE, D])
    gfin_col = din("gfin_col", [128, KC])
    c_ident = din("c_ident", [128, 128])
    c_tril = din("c_tril", [128, 128])
    c_trilT = din("c_trilT", [128, 128])
    c_invcnt = din("c_invcnt", [128, 4, 512])
    c_iota32 = din("c_iota32", [32, 128])
    flag_col = din("flag_col", [128, 1])
    pmb_col = din("pmb_col", [128, 1])
    out_d = nc.dram_tensor("out", [T, D], F32, kind="ExternalOutput").ap()

    dsc = lambda n, s, dt: nc.dram_tensor(n, s, dt).ap()
    xT = dsc("s_xT", [D, T], F32)
    hT_d = dsc("s_hT", [D, T], BF16)
    zp_d = dsc("s_zp", [512, T], F32)
    glu_d = dsc("s_glu", [512, T], BF16)
    qT_d = dsc("s_qT", [1024, T], BF16)
    CH = T // 2
    xkv = [[dsc(f"s_xkv{i}_{q}", [CH, 1024], BF16) for q in range(4)] for i in range(2)]
    kTa_all = [xk[0].rearrange("(a b) c -> a (b c)", b=T // 1024) for xk in xkv]
    kTb_all = [xk[1].rearrange("(a b) c -> a (b c)", b=T // 1024) for xk in xkv]
    va_all = [xk[2] for xk in xkv]
    vb_all = [xk[3] for xk in xkv]
    if split:
        gkv = [[dsc(f"s_gkv{i}_{q}", [2 * CH, 1024], BF16) for q in range(4)] for i in range(2)]
        xs_d = [dsc(f"s_xs{i}", [128, 320], F32) for i in range(2)]
        gs_d = [dsc(f"s_gs{i}", [256, 320], F32) for i in range(2)]
    R_gkv, R_gs, R_xsd = Res("gkv"), Res("gs"), Res("xsd")
    uT_d = dsc("s_uT", [512, T], BF16)
    gv_d = dsc("s_gv", [T, 512], BF16)
    yT_d = dsc("s_yT", [2560, T], BF16)
    R_hT, R_zp, R_glu, R_q, R_k, R_v, R_u, R_gv, R_y = [Res(n) for n in "hT zp glu q k v u gv y".split()]
    R_xTs = [Res(f"xT{i}") for i in range(KC)]
    R_in = Res("inputs")
    R_out = Res("out")
    xTv = xT.rearrange("(k p) t -> p k t", p=128)
    hTv = hT_d.rearrange("(k p) t -> p k t", p=128)

    es = ExitStack()
    with es:
        k = K(nc, es)
        pe, act, dve, pool, sp = k.pe, k.act, k.dve, k.pool, k.sp
        V, A, G = nc.vector, nc.scalar, nc.gpsimd

        ident, R_ident = k.sb("ident", [128, 128], F32)
        identb, R_identb = k.sb("identb", [128, 128], BF16)
        tril_b, R_trilb = k.sb("tril_b", [128, 128], BF16)
        tril_f, R_trilf = k.sb("tril_f", [128, 128], F32)
        trilT_f, R_trilT = k.sb("trilT_f", [128, 128], F32)
        ones_f, R_onesf = k.sb("ones_f", [128, 128], F32)
        ones_b, R_onesb = k.sb("ones_b", [128, 128], BF16)
        iota32, R_iota = k.sb("iota32", [32, 128], F32)
        ccol, R_ccol = k.sb("ccol", [128, KC], F32)
        modv, R_mod = k.sb("modv", [128, DEPTH, 96], F32)
        a1v, R_a1 = k.sb("a1v", [128, DEPTH, KC], F32)
        a2v, R_a2 = k.sb("a2v", [128, DEPTH, KC], F32)
        lf_sb, R_lf = k.sb("lf_sb", [128, NT, 8], F32)
        within, R_wi = k.sb("a_within", [128, NT, 8], F32)
        tot, R_tot = k.sb("a_tot", [128, NT, 8], F32)
        pinc, R_pinc = k.sb("a_pinc", [128, 8, NT], F32)
        negF, R_negF = k.sb("a_negF", [128, NT, 8], F32)
        flagc, R_flag = k.sb("flagc", [128, 1], F32)
        pmbc, R_pmb = k.sb("pmbc", [128, 1], F32)
        k.dma(sp, flagc[:], flag_col, [R_in], [R_flag], R_flag)
        k.dma(sp, pmbc[:], pmb_col, [R_in], [R_pmb], R_pmb)
        ccn = [0]

        def coll(in_ap, out_ap, R, W):
            csem = es.enter_context(nc.semaphore(f"ccsem{ccn[0]}"))
            ccn[0] += 1
            k._deps(pool, R, W)
            ins = G.collective_compute("AllGather", ALU.bypass, replica_groups=[[0, 1], [2, 3], [4, 5], [6, 7]],
                                       ins=[in_ap], outs=[out_ap])
            ins.then_inc(csem)
            k._mark((csem, 1), R, W)
        k.dma(sp, ident[:], c_ident, [R_in], [R_ident], R_ident)
        k.dma(pool, identb[:], c_ident, [R_in], [R_identb], R_identb)
        k.dma(pool, tril_b[:], c_tril, [R_in], [R_trilb], R_trilb)
        k.dma(sp, tril_f[:], c_tril, [R_in], [R_trilf], R_trilf)
        k.dma(sp, trilT_f[:], c_trilT, [R_in], [R_trilT], R_trilT)
        k.dma(sp, iota32[:], c_iota32, [R_in], [R_iota], R_iota)
        k.dma(sp, ccol[:], c_col, [R_in], [R_ccol], R_ccol)
        k.op(dve, lambda: V.memset(ones_f[:], 1.0), [], [R_onesf])
        k.op(dve, lambda: V.memset(ones_b[:], 1.0), [], [R_onesb])

        with ExitStack() as st:
            cact, R_cact = k.sb("cact", [128, KC], F32, st)
            wa = [k.sb(f"wa{i}", [128, KC, 512], F32, st) for i in range(2)]
            tmpA, R_tmpA = k.sb("tmpA", [128, 96], F32, st)
            k.op(act, lambda: A.activation(out=cact[:], in_=ccol[:], func=AF.Silu), [R_ccol], [R_cact])
            cnt = 0
            for l in range(nlayers):
                wv = w_ada[l].rearrange("(k p) c -> p k c", p=128)
                ps, R_ps = k.nextps()
                for blk in range(24):
                    wt, R_wt = wa[cnt % 2]
                    cnt += 1
                    k.dma(sp, wt[:], wv[:, :, blk * 512:(blk + 1) * 512], [R_in], [R_wt], R_wt)
                    for j in range(4):
                        col = blk * 4 + j
                        k.mm(ps[:, col:col + 1],
                             [(wt[:, kk, j * 128:(j + 1) * 128], cact[:, kk:kk + 1]) for kk in range(KC)],
                             [R_wt, R_cact], [R_ps])
                k.dma(sp, tmpA[:], b_ada_col[l], [R_in], [R_tmpA], R_tmpA)
                k.op(dve, lambda: V.tensor_tensor(out=modv[:, l, :], in0=ps[:, 0:96], in1=tmpA[:], op=ALU.add),
                     [R_ps, R_tmpA], [R_mod])
                k.dma(sp, tmpA[:, 0:16], gmix_col[l], [R_in], [R_tmpA], R_tmpA)
                k.dma(sp, tmpA[:, 16:32], gffn_col[l], [R_in], [R_tmpA], R_tmpA)
                k.op(dve, lambda: V.scalar_tensor_tensor(out=a1v[:, l, :], in0=modv[:, l, 16:32], scalar=1.0,
                                                         in1=tmpA[:, 0:16], op0=ALU.add, op1=ALU.mult),
                     [R_mod, R_tmpA], [R_a1])
                k.op(dve, lambda: V.scalar_tensor_tensor(out=a2v[:, l, :], in0=modv[:, l, 64:80], scalar=1.0,
                                                         in1=tmpA[:, 16:32], op0=ALU.add, op1=ALU.mult),
                     [R_mod, R_tmpA], [R_a2])
            k.barrier()
        SH1, GT1, SH2, GT2 = 0, 32, 48, 80

        with ExitStack() as st:
            xin = [k.sb(f"xin{i}", [128, 4, D], F32, st) for i in range(2)]
            xo = [k.sb(f"xo{i}", [128, KC, 512], F32, st) for i in range(2)]
            xv = x_in.rearrange("(n p) d -> p n d", p=128)
            for b in range(NB):
                xi, R_xi = xin[b % 2]
                xot, R_xo = xo[b % 2]
                k.dma(sp, xi[:], xv[:, b * 4:(b + 1) * 4, :], [R_in], [R_xi], R_xi)
                for kk in range(KC):
                    ps, R_ps = k.nextps()
                    for tt in range(4):
                        k.tr(ps[:, tt * 128:(tt + 1) * 128], xi[:, tt, kk * 128:(kk + 1) * 128], ident[:],
                             [R_xi, R_ident], [R_ps])
                    if kk % 2 == 0:
                        k.op(dve, lambda: V.tensor_copy(out=xot[:, kk, :], in_=ps[:]), [R_ps], [R_xo])
                    else:
                        k.op(act, lambda: A.copy(out=xot[:, kk, :], in_=ps[:]), [R_ps], [R_xo])
                k.dma(sp, xTv[:, :, b * 512:(b + 1) * 512], xot[:], [R_xo], R_xTs, R_xo)
            k.barrier()

        def norm_mod(st, l, sbi, avec, shoff, h_bf, R_hbf, h_f32=None, R_hf=None, store_h=False):
            xc = [k.sb(f"nm_x{i}", [128, 512], F32, st) for i in range(3)]
            sq = [k.sb(f"nm_sq{i}", [128, 512], F32, st) for i in range(2)]
            rs, R_rs = k.sb("nm_rs", [128, 512], F32, st)
            tm = [k.sb(f"nm_tm{i}", [128, 512], F32, st) for i in range(2)]
            xcn = 0
            for half in range(2):
                c0 = sbi * 1024 + half * 512
                ps, R_ps = k.nextps()
                for kk in range(KC):
                    xct, R_xc = xc[xcn % 3]
                    xcn += 1
                    k.dma(sp, xct[:], xT[kk * 128:(kk + 1) * 128, c0:c0 + 512], [R_xTs[kk]], [R_xc], R_xc)
                    sqt, R_sq = sq[kk % 2]
                    k.op(act, lambda: A.activation(out=sqt[:], in_=xct[:], func=AF.Square), [R_xc], [R_sq])
                    k.mm1(ps[:], ones_f[:], sqt[:], kk == 0, kk == KC - 1, [R_onesf, R_sq], [R_ps])
                k.op(dve, lambda: V.tensor_scalar(out=rs[:], in0=ps[:], scalar1=1.0 / D, scalar2=EPS,
                                                  op0=ALU.mult, op1=ALU.add), [R_ps], [R_rs])
                k.op(act, lambda: A.activation(out=rs[:], in_=rs[:], func=AF.Sqrt), [R_rs], [R_rs])
                k.op(dve, lambda: V.reciprocal(out=rs[:], in_=rs[:]), [R_rs], [R_rs])
                for kk in range(KC):
                    xct, R_xc = xc[xcn % 3]
                    xcn += 1
                    k.dma(sp, xct[:], xT[kk * 128:(kk + 1) * 128, c0:c0 + 512], [R_xTs[kk]], [R_xc], R_xc)
                    tmt, R_tm = tm[kk % 2]
                    k.op(dve, lambda: V.tensor_tensor(out=tmt[:], in0=xct[:], in1=rs[:], op=ALU.mult),
                         [R_xc, R_rs], [R_tm])
                    k.op(act, lambda: A.activation(out=h_bf[:, kk, half * 512:(half + 1) * 512], in_=tmt[:],
                                                   func=AF.Identity, bias=modv[:, l, shoff + kk:shoff + kk + 1],
                                                   scale=avec[:, l, kk:kk + 1]),
                         [R_tm, R_mod, R_a1, R_a2], [R_hbf])
                    if h_f32 is not None:
                        k.op(act, lambda: A.activation(out=h_f32[:, kk, half * 512:(half + 1) * 512], in_=tmt[:],
                                                       func=AF.Identity, bias=modv[:, l, shoff + kk:shoff + kk + 1],
                                                       scale=avec[:, l, kk:kk + 1]),
                             [R_tm, R_mod, R_a1, R_a2], [R_hf])
            if store_h:
                k.dma(sp, hTv[:, :, sbi * 1024:(sbi + 1) * 1024], h_bf[:], [R_hbf], [R_hT], R_hbf)

        def gelu_from_ps(ps, R_ps, outap, R_out, tA, R_tA, tB, R_tB, width=512):
            k.op(act, lambda: A.activation(out=tA[:, :width], in_=ps[:, :width], func=AF.Square), [R_ps], [R_tA])
            k.op(dve, lambda: V.tensor_scalar(out=tA[:, :width], in0=tA[:, :width], scalar1=0.044715, scalar2=1.0,
                                              op0=ALU.mult, op1=ALU.add), [R_tA], [R_tA])
            k.op(dve, lambda: V.tensor_tensor(out=tA[:, :width], in0=tA[:, :width], in1=ps[:, :width], op=ALU.mult),
                 [R_tA, R_ps], [R_tA])
            k.op(act, lambda: A.activation(out=tB[:, :width], in_=tA[:, :width], func=AF.Sigmoid,
                                           scale=1.5957691216057308), [R_tA], [R_tB])
            k.op(dve, lambda: V.tensor_tensor(out=outap, in0=tB[:, :width], in1=ps[:, :width], op=ALU.mult),
                 [R_tB, R_ps], [R_out])

        for l in range(nlayers):
            kTa, kTb, v_a, v_b = kTa_all[l % 2], kTb_all[l % 2], va_all[l % 2], vb_all[l % 2]
            with ExitStack() as st:
                hb, R_hb = k.sb("c_h", [128, KC, 1024], BF16, st)
                wsl = [k.sb(f"c_w{i}", [128, KC, 512], BF16, st) for i in range(3)]
                wf, R_wf = k.sb("c_wf", [128, KC, 8], BF16, st)
                sig, R_sig = k.sb("c_sig", [128, 4, 1024], F32, st)
                stb = [k.sb(f"c_stb{i}", [128, 1024], BF16, st) for i in range(3)]
                stf = [k.sb(f"c_stf{i}", [128, 1024], F32, st) for i in range(2)]
                tA, R_tA = k.sb("c_tA", [128, 512], F32, st)
                tB, R_tB = k.sb("c_tB", [128, 512], F32, st)
                tC, R_tC = k.sb("c_tC", [128, 512], F32, st)
                sc8, R_sc8 = k.sb("c_sc8", [128, 8], F32, st)
                fgb, R_fgb = k.sb("c_fgb", [128, 8], F32, st)
                glng, R_glng = k.sb("c_glng", [128, 512], F32, st)
                k.dma(sp, fgb[:], fgb_row[l], [R_in], [R_fgb], R_fgb)
                k.dma(sp, glng[:], glng_row[l], [R_in], [R_glng], R_glng)
                wcnt = [0]
                scnt = [0, 0]

                def loadw(c0, width=512):
                    wt, R_wt = wsl[wcnt[0] % 3]
                    wcnt[0] += 1
                    bi = c0 // 512 if c0 < O_F else 9 + (c0 - O_GU) // 512
                    k.dma(pool, wt[:], w_in_r[l, bi].rearrange("p (k c) -> p k c", c=512), [R_in], [R_wt], R_wt)
                    return wt, R_wt

                for sbi in range(NSB):
                    with ExitStack() as st2:
                        norm_mod(st2, l, sbi, a1v, SH1, hb, R_hb, store_h=True)
                        k.barrier()
                    t0 = sbi * 1024
                    fm = [(O_POOL, "pool", zp_d, 0), (O_CB, "cb", None, 0), (O_CA, "ca", glu_d, 0),
                          (O_Q, "cp", qT_d, 0), (O_Q + 512, "cp", qT_d, 512), (O_K, "cp", kTa, 0),
                          (O_K + 512, "cp", kTb, 0), (O_GU, "gelu", uT_d, 0)]
                    rmap = {id(zp_d): R_zp, id(glu_d): R_glu, id(qT_d): R_q, id(kTa): R_k, id(kTb): R_k, id(uT_d): R_u}
                    cjobs = [f_[0] for f_ in fm] + [O_V, O_V + 512, O_GV]
                    getc = prefetcher(cjobs, lambda j, c0_: loadw(c0_), 2)
                    for ji, (c0, kind, dst, roff) in enumerate(fm):
                        wt, R_wt = getc(ji)
                        for m in range(4):
                            if kind == "pool":
                                so, R_so = stf[scnt[1] % 2]
                                scnt[1] += 1
                            elif kind != "cb":
                                so, R_so = stb[scnt[0] % 3]
                                scnt[0] += 1
                            for half in range(2):
                                ps, R_ps = k.nextps()
                                k.mm(ps[:], [(wt[:, kk, m * 128:(m + 1) * 128], hb[:, kk, half * 512:(half + 1) * 512])
                                             for kk in range(KC)], [R_wt, R_hb], [R_ps])
                                hs = slice(half * 512, (half + 1) * 512)
                                if kind == "pool":
                                    k.op(act, lambda: A.copy(out=so[:, hs], in_=ps[:]), [R_ps], [R_so])
                                elif kind == "cb":
                                    k.op(act, lambda: A.activation(out=sig[:, m, hs], in_=ps[:], func=AF.Sigmoid),
                                         [R_ps], [R_sig])
                                elif kind == "ca":
                                    k.op(dve, lambda: V.tensor_tensor(out=so[:, hs], in0=ps[:], in1=sig[:, m, hs],
                                                                      op=ALU.mult), [R_ps, R_sig], [R_so])
                                elif kind == "cp":
                                    if half == 0:
                                        k.op(act, lambda: A.copy(out=so[:, hs], in_=ps[:]), [R_ps], [R_so])
                                    else:
                                        k.op(dve, lambda: V.tensor_copy(out=so[:, hs], in_=ps[:]), [R_ps], [R_so])
                                elif kind == "gelu":
                                    gelu_from_ps(ps, R_ps, so[:, hs], R_so, tA, R_tA, tB, R_tB)
                            if kind != "cb":
                                r0 = roff + m * 128
                                k.dma(sp, dst[r0:r0 + 128, t0:t0 + 1024], so[:], [R_so], [rmap[id(dst)]], R_so)
                    for ji, (c0, kind, coff) in enumerate([(O_V, "v", 0), (O_V + 512, "v", 512), (O_GV, "gv", 0)]):
                        wt, R_wt = getc(len(fm) + ji)
                        for tt in range(8):
                            ps, R_ps = k.nextps()
                            k.mm(ps[:], [(hb[:, kk, tt * 128:(tt + 1) * 128], wt[:, kk, :]) for kk in range(KC)],
                                 [R_wt, R_hb], [R_ps])
                            so, R_so = stb[scnt[0] % 3]
                            scnt[0] += 1
                            r0 = t0 + tt * 128
                            if kind == "v":
                                k.op(act, lambda: A.copy(out=so[:, 0:512], in_=ps[:]), [R_ps], [R_so])
                                vdst, rr = (v_a, r0) if r0 < CH else (v_b, r0 - CH)
                                k.dma(sp, vdst[rr:rr + 128, coff:coff + 512], so[:, 0:512], [R_so], [R_v], R_so)
                            else:
                                gelu_from_ps(ps, R_ps, tC[:], R_tC, tA, R_tA, tB, R_tB)
                                k.op(dve, lambda: V.memset(sc8[:, 0:1], 0.0), [], [R_sc8])
                                k.op(act, lambda: A.activation(out=tA[:], in_=tC[:], func=AF.Square,
                                                               accum_out=sc8[:, 0:1]), [R_tC], [R_tA, R_sc8])
                                k.op(dve, lambda: V.tensor_scalar(out=sc8[:, 1:2], in0=sc8[:, 0:1], scalar1=1.0 / 512,
                                                                  scalar2=EPS, op0=ALU.mult, op1=ALU.add),
                                     [R_sc8], [R_sc8])
                                k.op(act, lambda: A.activation(out=sc8[:, 2:3], in_=sc8[:, 1:2], func=AF.Sqrt),
                                     [R_sc8], [R_sc8])
                                k.op(dve, lambda: V.reciprocal(out=sc8[:, 3:4], in_=sc8[:, 2:3]), [R_sc8], [R_sc8])
                                k.op(dve, lambda: V.scalar_tensor_tensor(out=so[:, 0:512], in0=tC[:],
                                                                         scalar=sc8[:, 3:4], in1=glng[:],
                                                                         op0=ALU.mult, op1=ALU.mult),
                                     [R_tC, R_sc8, R_glng], [R_so])
                                k.dma(sp, gv_d[r0:r0 + 128, :], so[:, 0:512], [R_so], [R_gv], R_so)
                    k.dma(pool, wf[:], wf_r[l].rearrange("p (k c) -> p k c", c=8), [R_in], [R_wf], R_wf)
                    for tt in range(8):
                        ps, R_ps = k.nextps()
                        k.mm(ps[:, 0:8], [(hb[:, kk, tt * 128:(tt + 1) * 128], wf[:, kk, :]) for kk in range(KC)],
                             [R_wf, R_hb], [R_ps])
                        gt = sbi * 8 + tt
                        k.op(dve, lambda: V.tensor_tensor(out=sc8[:], in0=ps[:, 0:8], in1=fgb[:], op=ALU.add),
                             [R_ps, R_fgb], [R_sc8])
                        k.op(act, lambda: A.activation(out=sc8[:], in_=sc8[:], func=AF.Exp, scale=-1.0),
                             [R_sc8], [R_sc8])
                        k.op(act, lambda: A.activation(out=sc8[:], in_=sc8[:], func=AF.Ln, bias=1.0, scale=1.0),
                             [R_sc8], [R_sc8])
                        k.op(dve, lambda: V.tensor_scalar(out=lf_sb[:, gt, :], in0=sc8[:], scalar1=-1.0, scalar2=None,
                                                          op0=ALU.mult), [R_sc8], [R_lf])
                k.barrier()

            with ExitStack() as st:
                lfv = lf_sb[:].rearrange("p n h -> p (n h)")
                for c0 in range(0, NT * 8, 512):
                    w_ = min(512, NT * 8 - c0)
                    ps, R_ps = k.nextps()
                    k.mm(ps[:, 0:w_], [(tril_f[:], lfv[:, c0:c0 + w_])], [R_trilf, R_lf], [R_ps])
                    k.op(dve, lambda: V.tensor_copy(out=within[:].rearrange("p n h -> p (n h)")[:, c0:c0 + w_],
                                                    in_=ps[:, 0:w_]), [R_ps], [R_wi])
                    ps, R_ps = k.nextps()
                    k.mm(ps[:, 0:w_], [(ones_f[:], lfv[:, c0:c0 + w_])], [R_onesf, R_lf], [R_ps])
                    k.op(dve, lambda: V.tensor_copy(out=tot[:].rearrange("p n h -> p (n h)")[:, c0:c0 + w_],
                                                    in_=ps[:, 0:w_]), [R_ps], [R_tot])
                k.op(dve, lambda: V.tensor_copy(out=pinc[:, :, 0], in_=tot[:, 0, :]), [R_tot], [R_pinc])
                k.op(dve, lambda: V.tensor_scalar(out=negF[:, 0, :], in0=within[:, 0, :], scalar1=-1.0, scalar2=None,
                                                  op0=ALU.mult), [R_wi], [R_negF])
                for j in range(1, NT):
                    k.op(dve, lambda: V.tensor_tensor(out=pinc[:, :, j], in0=pinc[:, :, j - 1], in1=tot[:, j, :],
                                                      op=ALU.add), [R_pinc, R_tot], [R_pinc])
                    k.op(dve, lambda: V.scalar_tensor_tensor(out=negF[:, j, :], in0=within[:, j, :], scalar=-1.0,
                                                             in1=pinc[:, :, j - 1], op0=ALU.mult, op1=ALU.subtract),
                         [R_wi, R_pinc], [R_negF])
                if split:
                    xs, R_xs = k.sb("x_xs", [128, 320], F32, st)
                    hz, R_hz = k.sb("x_hz", [128, 4, 15], F32, st)
                    hg, R_hg = k.sb("x_hg", [128, 4, 30], BF16, st)
                    k.op(dve, lambda: V.memset(xs[:], 0.0), [], [R_xs])
                    k.op(dve, lambda: V.tensor_copy(out=xs[:, 0:NT * 8], in_=negF[:].rearrange("p n h -> p (n h)")),
                         [R_negF], [R_xs])
                    k.op(dve, lambda: V.tensor_copy(out=xs[:, 128:136], in_=pinc[:, :, NT - 1]), [R_pinc], [R_xs])
                    k.dma(sp, hz[:], zp_d.rearrange("(g p) t -> p g t", p=128)[:, :, T - 15:T], [R_zp], [R_hz], R_hz)
                    k.dma(sp, hg[:], glu_d.rearrange("(g p) t -> p g t", p=128)[:, :, T - 30:T], [R_glu], [R_hg], R_hg)
                    k.op(dve, lambda: V.tensor_copy(out=xs[:, 136:196].rearrange("p (g t) -> p g t", g=4), in_=hz[:]),
                         [R_hz], [R_xs])
                    k.op(dve, lambda: V.tensor_copy(out=xs[:, 196:316].rearrange("p (g t) -> p g t", g=4), in_=hg[:]),
                         [R_hg], [R_xs])
                    k.dma(sp, xs_d[l % 2][:, :], xs[:], [R_xs], [R_xsd], R_xs)
                    coll(xs_d[l % 2][:, :], gs_d[l % 2][:, :], [R_xsd], [R_gs])
                    for q in range(4):
                        coll(xkv[l % 2][q][:, :], gkv[l % 2][q][:, :], [R_k, R_v], [R_gkv])
                k.barrier()

            with ExitStack() as st:
                zb = [k.sb(f"p_z{i}", [128, 4, 527], F32, st) for i in range(2)]
                pa, R_pa = k.sb("p_a", [128, 527], F32, st)
                pb, R_pb = k.sb("p_b", [128, 527], F32, st)
                mx = [k.sb(f"p_mx{i}", [128, 512], BF16, st) for i in range(2)]
                pw, R_pw = k.sb("p_w", [128, 4, 128], BF16, st)
                psc, R_psc = k.sb("p_sc", [128, 4], F32, st)
                icn, R_icn = k.sb("p_icn", [128, 4, 512], F32, st)
                so = [k.sb(f"p_so{i}", [128, 4, 512], BF16, st) for i in range(2)]
                k.dma(pool, pw[:], pool_w[l].rearrange("g c d -> c g d"), [R_in], [R_pw], R_pw)
                k.dma(sp, psc[:], pscale_col[l], [R_in], [R_psc], R_psc)
                k.dma(sp, icn[:], c_invcnt, [R_in], [R_icn], R_icn)
                zpv = zp_d.rearrange("(g p) t -> p g t", p=128)
                yv = yT_d.rearrange("(g p) t -> p g t", p=128)
                for b in range(NB):
                    z, R_z = zb[b % 2]
                    sot, R_so = so[b % 2]
                    if b == 0:
                        if split:
                            hz2, R_hz2 = k.sb("p_hz2", [128, 60], F32, st)
                            k.dma(sp, hz2[:], gs_d[l % 2][0:128, 136:196], [R_gs], [R_hz2], R_hz2)
                            k.op(dve, lambda: V.tensor_scalar(out=z[:, :, 0:15],
                                                              in0=hz2[:].rearrange("p (g t) -> p g t", g=4),
                                                              scalar1=flagc[:, 0:1], scalar2=None, op0=ALU.mult),
                                 [R_hz2, R_flag], [R_z])
                        else:
                            k.op(dve, lambda: V.memset(z[:, :, 0:15], 0.0), [], [R_z])
                        k.dma(sp, z[:, :, 15:527], zpv[:, :, 0:512], [R_zp], [R_z], R_z)
                    else:
                        k.dma(sp, z[:, :, :], zpv[:, :, b * 512 - 15:b * 512 + 512], [R_zp], [R_z], R_z)
                    for g in range(4):
                        src = z[:, g, :]
                        bufs = [(pa, R_pa), (pb, R_pb)]
                        cur, R_cur = None, R_z
                        for s in range(g + 1):
                            sh = 1 << s
                            lo = 2 * sh - 1
                            dstb, R_d = bufs[s % 2]
                            srcap = src if cur is None else cur
                            k.op(dve, lambda: V.tensor_tensor(out=dstb[:, lo:527], in0=srcap[:, lo:527],
                                                              in1=srcap[:, lo - sh:527 - sh], op=ALU.add),
                                 [R_cur], [R_d])
                            cur, R_cur = dstb, R_d
                        mt, R_mt = mx[g % 2]
                        w = 2 << g
                        if b == 0:
                            k.op(dve, lambda: V.tensor_tensor(out=cur[:, 15:527], in0=cur[:, 15:527], in1=icn[:, g, :],
                                                              op=ALU.mult), [R_cur, R_icn], [R_cur])
                            k.op(dve, lambda: V.tensor_tensor(out=mt[:], in0=cur[:, 15:527], in1=z[:, g, 15:527],
                                                              op=ALU.subtract), [R_cur, R_z], [R_mt])
                        else:
                            k.op(dve, lambda: V.scalar_tensor_tensor(out=mt[:], in0=cur[:, 15:527], scalar=1.0 / w,
                                                                     in1=z[:, g, 15:527], op0=ALU.mult,
                                                                     op1=ALU.subtract), [R_cur, R_z], [R_mt])
                        ps, R_ps = k.nextps()
                        k.mm(ps[:], [(pw[:, g, :], mt[:])], [R_pw, R_mt], [R_ps])
                        k.op(act, lambda: A.activation(out=sot[:, g, :], in_=ps[:], func=AF.Identity, bias=0.0,
                                                       scale=psc[:, g:g + 1]), [R_ps, R_psc], [R_so])
                    k.dma(sp, yv[:, 0:4, b * 512:(b + 1) * 512], sot[:], [R_so], [R_y], R_so)
                k.barrier()

            with ExitStack() as st:
                gb = [k.sb(f"v_g{i}", [128, 4, 542], BF16, st) for i in range(2)]
                dg, R_dg = k.sb("v_dg", [128, 124, 128], BF16, st)
                cw, R_cw = k.sb("v_cw", [128, 4, 31], F32, st)
                cb, R_cb = k.sb("v_cb", [128, 4], F32, st)
                lg, R_lg = k.sb("v_lg", [128, 4], F32, st)
                lb, R_lb = k.sb("v_lb", [128, 4], F32, st)
                yc, R_yc = k.sb("v_yc", [128, 4, 512], F32, st)
                ysq = [k.sb(f"v_ysq{i}", [128, 512], F32, st) for i in range(2)]
                mean, R_mean = k.sb("v_mean", [128, 512], F32, st)
                rstd, R_rstd = k.sb("v_rstd", [128, 512], F32, st)
                tq, R_tq = k.sb("v_tq", [128, 512], F32, st)
                so = [k.sb(f"v_so{i}", [128, 4, 512], BF16, st) for i in range(2)]
                k.dma(sp, cw[:], convw_col[l], [R_in], [R_cw], R_cw)
                k.dma(sp, cb[:], convb_col[l], [R_in], [R_cb], R_cb)
                k.dma(sp, lg[:], clng_col[l], [R_in], [R_lg], R_lg)
                k.dma(sp, lb[:], clnb_col[l], [R_in], [R_lb], R_lb)
                for c in range(4):
                    for j in range(31):
                        k.op(dve, lambda: V.tensor_scalar(out=dg[:, c * 31 + j, :], in0=identb[:],
                                                          scalar1=cw[:, c, j:j + 1], scalar2=None, op0=ALU.mult),
                             [R_identb, R_cw], [R_dg])
                gv_ = glu_d.rearrange("(g p) t -> p g t", p=128)
                yv = yT_d.rearrange("(g p) t -> p g t", p=128)
                for b in range(NB):
                    g_, R_g = gb[b % 2]
                    sot, R_so = so[b % 2]
                    if b == 0:
                        if split:
                            hg2, R_hg2 = k.sb("v_hg2", [128, 120], F32, st)
                            k.dma(sp, hg2[:], gs_d[l % 2][0:128, 196:316], [R_gs], [R_hg2], R_hg2)
                            k.op(dve, lambda: V.tensor_scalar(out=g_[:, :, 0:30],
                                                              in0=hg2[:].rearrange("p (g t) -> p g t", g=4),
                                                              scalar1=flagc[:, 0:1], scalar2=None, op0=ALU.mult),
                                 [R_hg2, R_flag], [R_g])
                        else:
                            k.op(dve, lambda: V.memset(g_[:, :, 0:30], 0.0), [], [R_g])
                        k.dma(sp, g_[:, :, 30:542], gv_[:, :, 0:512], [R_glu], [R_g], R_g)
                    else:
                        k.dma(sp, g_[:, :, :], gv_[:, :, b * 512 - 30:b * 512 + 512], [R_glu], [R_g], R_g)
                    psA, R_psA = k.nextps()
                    psB, R_psB = k.nextps()
                    for c in range(4):
                        ps, R_ps = k.nextps()
                        k.mm(ps[:], [(dg[:, c * 31 + j, :], g_[:, c, j:j + 512]) for j in range(31)],
                             [R_dg, R_g], [R_ps])
                        k.op(act, lambda: A.activation(out=yc[:, c, :], in_=ps[:], func=AF.Identity,
                                                       bias=cb[:, c:c + 1], scale=1.0), [R_ps, R_cb], [R_yc])
                        yq, R_yq = ysq[c % 2]
                        k.op(act, lambda: A.activation(out=yq[:], in_=yc[:, c, :], func=AF.Square), [R_yc], [R_yq])
                        k.mm1(psA[:], ones_f[:], yc[:, c, :], c == 0, c == 3, [R_onesf, R_yc], [R_psA])
                        k.mm1(psB[:], ones_f[:], yq[:], c == 0, c == 3, [R_onesf, R_yq], [R_psB])
                    k.op(dve, lambda: V.tensor_scalar(out=mean[:], in0=psA[:], scalar1=1.0 / 512, scalar2=None,
                                                      op0=ALU.mult), [R_psA], [R_mean])
                    k.op(dve, lambda: V.tensor_tensor(out=tq[:], in0=mean[:], in1=mean[:], op=ALU.mult),
                         [R_mean], [R_tq])
                    k.op(dve, lambda: V.scalar_tensor_tensor(out=rstd[:], in0=psB[:], scalar=1.0 / 512, in1=tq[:],
                                                             op0=ALU.mult, op1=ALU.subtract), [R_psB, R_tq], [R_rstd])
                    k.op(dve, lambda: V.tensor_scalar(out=rstd[:], in0=rstd[:], scalar1=EPS, scalar2=None,
                                                      op0=ALU.add), [R_rstd], [R_rstd])
                    k.op(act, lambda: A.activation(out=rstd[:], in_=rstd[:], func=AF.Sqrt), [R_rstd], [R_rstd])
                    k.op(dve, lambda: V.reciprocal(out=rstd[:], in_=rstd[:]), [R_rstd], [R_rstd])
                    for c in range(4):
                        k.op(dve, lambda: V.tensor_tensor(out=yc[:, c, :], in0=yc[:, c, :], in1=mean[:],
                                                          op=ALU.subtract), [R_yc, R_mean], [R_yc])
                        k.op(dve, lambda: V.tensor_tensor(out=yc[:, c, :], in0=yc[:, c, :], in1=rstd[:],
                                                          op=ALU.mult), [R_yc, R_rstd], [R_yc])
                        k.op(act, lambda: A.activation(out=sot[:, c, :], in_=yc[:, c, :], func=AF.Silu,
                                                       bias=lb[:, c:c + 1], scale=lg[:, c:c + 1]),
                             [R_yc, R_lb, R_lg], [R_so])
                    k.dma(sp, yv[:, 4:8, b * 512:(b + 1) * 512], sot[:], [R_so], [R_y], R_so)
                k.barrier()

            with ExitStack() as st:
                npair = NT * (NT + 1) // 2
                bias_t, R_bias = k.sb("a_bias", [128, npair], F32, st)
                kt = [k.sb(f"a_k{i}", [128, T], BF16, st) for i in range(2)]
                qt = [k.sb(f"a_q{i}", [128, T], BF16, st) for i in range(2)]
                vt = [k.sb(f"a_v{i}", [128, NT, 128], BF16, st) for i in range(2)]
                pT = [k.sb(f"a_p{i}", [128, 512], BF16, st) for i in range(3)]
                rc, R_rc = k.sb("a_rc", [128, 512], F32, st)
                so = [k.sb(f"a_so{i}", [128, 512], BF16, st) for i in range(2)]
                if split:
                    ktp = [k.sb(f"a_kp{i}", [128, T], BF16, st) for i in range(2)]
                    vtp = [k.sb(f"a_vp{i}", [128, NT, 128], BF16, st) for i in range(2)]
                    bias_p, R_biasp = k.sb("a_biasp", [128, NT * NT], F32, st)
                    gsm, R_gsm = k.sb("a_gsm", [128, 136], F32, st)
                    tp, R_tp = k.sb("a_tp", [128, 8], F32, st)
                    cv, R_cv = k.sb("a_cv", [128, NT, 8], F32, st)
                    k.dma(sp, gsm[:], gs_d[l % 2][0:128, 0:136], [R_gs], [R_gsm], R_gsm)
                    k.op(dve, lambda: V.tensor_scalar(out=tp[:], in0=gsm[:, 128:136], scalar1=flagc[:, 0:1],
                                                      scalar2=pmbc[:, 0:1], op0=ALU.mult, op1=ALU.add),
                         [R_gsm, R_flag, R_pmb], [R_tp])
                    for i in range(NT):
                        k.op(dve, lambda: V.tensor_tensor(out=cv[:, i, :], in0=gsm[:, i * 8:(i + 1) * 8], in1=tp[:],
                                                          op=ALU.add), [R_gsm, R_tp], [R_cv])
                    gka = gkv[l % 2][0][0:CH, :].rearrange("(a b) c -> a (b c)", b=T // 1024)
                    gkb = gkv[l % 2][1][0:CH, :].rearrange("(a b) c -> a (b c)", b=T // 1024)
                    gva = gkv[l % 2][2][0:CH, :].rearrange("(n p) c -> p n c", p=128)
                    gvb_ = gkv[l % 2][3][0:CH, :].rearrange("(n p) c -> p n c", p=128)
                yv = yT_d.rearrange("(g p) t -> p g t", p=128)
                vva = v_a.rearrange("(n p) c -> p n c", p=128)
                vvb = v_b.rearrange("(n p) c -> p n c", p=128)
                NH = NT // 2
                pcnt = 0
                for h in range(8):
                    ktt, R_kt = kt[h % 2]
                    qtt, R_qt = qt[h % 2]
                    vtt, R_vt = vt[h % 2]
                    ksrc_d = kTa if h < 4 else kTb
                    hr = (h % 4) * 128
                    k.dma(sp, ktt[:], ksrc_d[hr:hr + 128, :], [R_k], [R_kt], R_kt)
                    k.dma(sp, qtt[:], qT_d[h * 128:(h + 1) * 128, :], [R_q], [R_qt], R_qt)
                    k.dma(sp, vtt[:, 0:NH, :], vva[:, :, h * 128:(h + 1) * 128], [R_v], [R_vt], R_vt)
                    k.dma(sp, vtt[:, NH:NT, :], vvb[:, :, h * 128:(h + 1) * 128], [R_v], [R_vt], R_vt)
                    if split:
                        ktpt, R_ktp = ktp[h % 2]
                        vtpt, R_vtp = vtp[h % 2]
                        gk = gka if h < 4 else gkb
                        k.dma(sp, ktpt[:], gk[hr:hr + 128, :], [R_gkv], [R_ktp], R_ktp)
                        k.dma(sp, vtpt[:, 0:NH, :], gva[:, :, h * 128:(h + 1) * 128], [R_gkv], [R_vtp], R_vtp)
                        k.dma(sp, vtpt[:, NH:NT, :], gvb_[:, :, h * 128:(h + 1) * 128], [R_gkv], [R_vtp], R_vtp)
                        for i in range(NT):
                            k.op(dve, lambda: V.tensor_scalar(out=bias_p[:, i * NT:(i + 1) * NT], in0=pinc[:, h, 0:NT],
                                                              scalar1=cv[:, i, h:h + 1], scalar2=None, op0=ALU.add),
                                 [R_pinc, R_cv], [R_biasp])
                    offs = []
                    o = 0
                    for i in range(NT):
                        offs.append(o)
                        k.op(dve, lambda: V.tensor_scalar(out=bias_t[:, o:o + NT - i], in0=pinc[:, h, i:NT],
                                                          scalar1=negF[:, i, h:h + 1], scalar2=None, op0=ALU.add),
                             [R_pinc, R_negF], [R_bias])
                        o += NT - i
                    for qb in range(NB):
                        psO, R_psO = k.ps[3 + qb % 2], k.psr[3 + qb % 2]
                        psD, R_psD = k.ps[5 + qb % 2], k.psr[5 + qb % 2]
                        steps = ([("p", i) for i in range(NT)] if split else []) + [("o", i) for i in range(4 * qb + 4)]
                        for si, (kind, i) in enumerate(steps):
                            first, last = si == 0, si == len(steps) - 1
                            kk_ = i - 4 * qb if kind == "o" else -1
                            j0 = max(kk_, 0)
                            cs = slice(j0 * 128, 512)
                            psS, R_psS = k.ps[pcnt % 3], k.psr[pcnt % 3]
                            pt, R_pt = pT[pcnt % 3]
                            pcnt += 1
                            if kind == "p":
                                ksrc, R_ks, vsrc, R_vs = ktpt, R_ktp, vtpt, R_vtp
                            else:
                                ksrc, R_ks, vsrc, R_vs = ktt, R_kt, vtt, R_vt
                            k.mm(psS[:, cs], [(ksrc[:, i * 128:(i + 1) * 128],
                                               qtt[:, qb * 512 + j0 * 128:(qb + 1) * 512])], [R_ks, R_qt], [R_psS])
                            for jj in range(j0, 4):
                                j = 4 * qb + jj
                                js = slice(jj * 128, (jj + 1) * 128)
                                if kind == "p":
                                    bap, R_b = bias_p[:, i * NT + j:i * NT + j + 1], R_biasp
                                else:
                                    bi = offs[i] + (j - i)
                                    bap, R_b = bias_t[:, bi:bi + 1], R_bias
                                k.op(act, lambda: A.activation(out=pt[:, js], in_=psS[:, js], func=AF.Exp,
                                                               bias=bap, scale=0.08838834764831845),
                                     [R_psS, R_b], [R_pt])
                                if kind == "o" and j == i:
                                    k.op(pool, lambda: G.tensor_tensor(out=pt[:, js], in0=pt[:, js], in1=tril_b[:],
                                                                       op=ALU.mult), [R_pt, R_trilb], [R_pt])
                            k.mm1(psO[:, cs], vsrc[:, i, :], pt[:, cs], first, last, [R_vs, R_pt], [R_psO])
                            k.mm1(psD[:, cs], ones_b[:], pt[:, cs], first, last, [R_onesb, R_pt], [R_psD])
                        sot, R_so = so[qb % 2]
                        k.op(dve, lambda: V.reciprocal(out=rc[:], in_=psD[:]), [R_psD], [R_rc])
                        k.op(dve, lambda: V.tensor_tensor(out=sot[:], in0=psO[:], in1=rc[:], op=ALU.mult),
                             [R_psO, R_rc], [R_so])
                        k.dma(sp, yv[:, 8 + h, qb * 512:(qb + 1) * 512], sot[:], [R_so], [R_y], R_so)
                k.psi = 0
                k.barrier()

            with ExitStack() as st:
                wsf, R_wsf = k.sb("g_wsf", [128, 4, 128], F32, st)
                wmT, R_wmT = k.sb("g_wmT", [128, 4, 128], BF16, st)
                bsr, R_bsr = k.sb("g_bsr", [128, 512], F32, st)
                gvb = [k.sb(f"g_gv{i}", [128, 4, 512], BF16, st) for i in range(2)]
                ub = [k.sb(f"g_u{i}", [128, 4, 512], BF16, st) for i in range(2)]
                tg, R_tg = k.sb("g_t", [128, 512], F32, st)
                so = [k.sb(f"g_so{i}", [128, 4, 512], BF16, st) for i in range(2)]
                k.dma(sp, wsf[:], gmlp_ws[l].rearrange("g t s -> t g s"), [R_in], [R_wsf], R_wsf)
                k.dma(sp, bsr[:], gbs_row[l], [R_in], [R_bsr], R_bsr)
                for g in range(4):
                    k.op(dve, lambda: V.tensor_tensor(out=wsf[:, g, :], in0=wsf[:, g, :], in1=trilT_f[:], op=ALU.mult),
                         [R_wsf, R_trilT], [R_wsf])
                ps, R_ps = k.nextps()
                for g in range(4):
                    k.tr(ps[:, g * 128:(g + 1) * 128], wsf[:, g, :], ident[:], [R_wsf, R_ident], [R_ps])
                k.op(dve, lambda: V.tensor_copy(out=wmT[:].rearrange("p g t -> p (g t)"), in_=ps[:]), [R_ps], [R_wmT])
                gvv = gv_d.rearrange("(n p) c -> p n c", p=128)
                uv = uT_d.rearrange("(g p) t -> p g t", p=128)
                yv = yT_d.rearrange("(g p) t -> p g t", p=128)
                for b in range(NB):
                    gvt, R_gvt = gvb[b % 2]
                    ut, R_ut = ub[b % 2]
                    sot, R_so = so[b % 2]
                    k.dma(sp, gvt[:], gvv[:, b * 4:(b + 1) * 4, :], [R_gv], [R_gvt], R_gvt)
                    k.dma(sp, ut[:], uv[:, :, b * 512:(b + 1) * 512], [R_u], [R_ut], R_ut)
                    for n in range(4):
                        ps, R_ps = k.nextps()
                        for g in range(4):
                            k.mm(ps[:, g * 128:(g + 1) * 128], [(gvt[:, n, g * 128:(g + 1) * 128], wmT[:, g, :])],
                                 [R_gvt, R_wmT], [R_ps])
                        k.op(dve, lambda: V.tensor_tensor(out=tg[:], in0=ps[:], in1=bsr[:], op=ALU.add),
                             [R_ps, R_bsr], [R_tg])
                        k.op(dve, lambda: V.tensor_tensor(out=sot[:, :, n * 128:(n + 1) * 128],
                                                          in0=tg[:].rearrange("p (g t) -> p g t", g=4),
                                                          in1=ut[:, :, n * 128:(n + 1) * 128], op=ALU.mult),
                             [R_tg, R_ut], [R_so])
                    k.dma(sp, yv[:, 16:20, b * 512:(b + 1) * 512], sot[:], [R_so], [R_y], R_so)
                k.barrier()

            with ExitStack() as st:
                hb, R_hb = k.sb("e_h", [128, KC, 1024], BF16, st)
                yb, R_yb = k.sb("e_y", [128, 20, 1024], BF16, st)
                mg, R_mg = k.sb("e_mg", [128, KC, 1024], BF16, st)
                wg = [k.sb(f"e_wg{i}", [128, 4, KC, 128], BF16, st) for i in range(2)]
                wb = [k.sb(f"e_wb{i}", [128, 20, 128], BF16, st) for i in range(2)]
                wo = [k.sb(f"e_wo{i}", [128, KC, 128], BF16, st) for i in range(2)]
                bg, R_bg = k.sb("e_bg", [128, 64], F32, st)
                gs = [k.sb(f"e_gs{i}", [128, 512], F32, st) for i in range(2)]
                acc = [k.sb(f"e_acc{i}", [128, 512], F32, st) for i in range(2)]
                tmp = [k.sb(f"e_tmp{i}", [128, 512], F32, st) for i in range(2)]
                xo = [k.sb(f"e_xo{i}", [128, 1024], F32, st) for i in range(2)]
                k.dma(sp, bg[:], bgate_col[l], [R_in], [R_bg], R_bg)
                yv = yT_d.rearrange("(g p) t -> p g t", p=128)
                brk = [(0, 4), (4, 8), (8, 16), (16, 20)]
                cnt = 0
                for sbi in range(NSB):
                    t0 = sbi * 1024
                    k.dma(sp, hb[:], hTv[:, :, t0:t0 + 1024], [R_hT], [R_hb], R_hb)
                    k.dma(sp, yb[:], yv[:, :, t0:t0 + 1024], [R_y], [R_yb], R_yb)
                    def issue_g(j, m_):
                        wgt, R_wg = wg[m_ % 2]
                        wbt, R_wb = wb[m_ % 2]
                        k.dma(pool, wgt[:], w_gate_r[l, m_].rearrange("p (i k c) -> p i k c", i=4, k=KC),
                              [R_in], [R_wg], R_wg)
                        k.dma(pool, wbt[:], w_branch_r[l, m_].rearrange("p (k c) -> p k c", c=128),
                              [R_in], [R_wb], R_wb)
                        return wgt, R_wg, wbt, R_wb
                    getg = prefetcher(list(range(KC)), issue_g, 1)

                    def issue_o(j, m_):
                        wot, R_wo = wo[m_ % 2]
                        k.dma(pool, wot[:], w_o_r[l, m_].rearrange("p (k c) -> p k c", c=128), [R_in], [R_wo], R_wo)
                        return wot, R_wo
                    geto = prefetcher(list(range(KC)), issue_o, 1)
                    for m in range(KC):
                        wgt, R_wg, wbt, R_wb = getg(m)
                        for half in range(2):
                            hs = slice(half * 512, (half + 1) * 512)
                            at, R_at = acc[half]
                            for i in range(4):
                                psg, R_psg = k.nextps()
                                k.mm(psg[:], [(wgt[:, i, kk, :], hb[:, kk, hs]) for kk in range(KC)],
                                     [R_wg, R_hb], [R_psg])
                                gt_, R_gs = gs[cnt % 2]
                                tt_, R_tp = tmp[cnt % 2]
                                cnt += 1
                                k.op(act, lambda: A.activation(out=gt_[:], in_=psg[:], func=AF.Sigmoid,
                                                               bias=bg[:, i * 16 + m:i * 16 + m + 1], scale=1.0),
                                     [R_psg, R_bg], [R_gs])
                                psb, R_psb = k.nextps()
                                k0, k1 = brk[i]
                                k.mm(psb[:], [(wbt[:, kk, :], yb[:, kk, hs]) for kk in range(k0, k1)],
                                     [R_wb, R_yb], [R_psb])
                                if i == 0:
                                    k.op(dve, lambda: V.tensor_tensor(out=at[:], in0=psb[:], in1=gt_[:], op=ALU.mult),
                                         [R_psb, R_gs], [R_at])
                                else:
                                    k.op(dve, lambda: V.tensor_tensor(out=tt_[:], in0=psb[:], in1=gt_[:], op=ALU.mult),
                                         [R_psb, R_gs], [R_tp])
                                    if i < 3:
                                        k.op(pool, lambda: G.tensor_tensor(out=at[:], in0=at[:], in1=tt_[:], op=ALU.add),
                                             [R_at, R_tp], [R_at])
                                    else:
                                        k.op(pool, lambda: G.tensor_tensor(out=mg[:, m, hs], in0=at[:], in1=tt_[:],
                                                                           op=ALU.add), [R_at, R_tp], [R_mg])
                    for m in range(KC):
                        wot, R_wo = geto(m)
                        xot, R_xo = xo[m % 2]
                        k.dma(sp, xot[:], xT[m * 128:(m + 1) * 128, t0:t0 + 1024], [R_xTs[m]], [R_xo], R_xo)
                        for half in range(2):
                            hs = slice(half * 512, (half + 1) * 512)
                            ps, R_ps = k.nextps()
                            k.mm(ps[:], [(wot[:, kk, :], mg[:, kk, hs]) for kk in range(KC)], [R_wo, R_mg], [R_ps])
                            k.op(dve, lambda: V.scalar_tensor_tensor(out=xot[:, hs], in0=ps[:],
                                                                     scalar=modv[:, l, GT1 + m:GT1 + m + 1],
                                                                     in1=xot[:, hs], op0=ALU.mult, op1=ALU.add),
                                 [R_ps, R_mod, R_xo], [R_xo])
                        k.dma(sp, xT[m * 128:(m + 1) * 128, t0:t0 + 1024], xot[:], [R_xo], [R_xTs[m]], R_xo)
                k.barrier()

            if not do_moe:
                continue
            with ExitStack() as st:
                yacc, R_yacc = k.sb("f_yacc", [128, KC, 1024], F32, st)
                h2, R_h2 = k.sb("f_h2", [128, KC, 1024], BF16, st)
                gbc = [k.sb(f"f_gbc{i}", [128, 1024], F32, st) for i in range(2)]
                GT, R_GT = k.sb("f_GT", [32, 1024], F32, st)
                sel = [k.sb(f"f_sel{i}", [32, 128], F32, st) for i in range(2)]
                bup, R_bup = k.sb("f_bup", [128, NE * 12], F32, st)
                k.dma(sp, bup[:], bup_col[l], [R_in], [R_bup], R_bup)
                ucnt = 0
                dcnt = 0
                ecnt = 0
                for sbi in range(NSB):
                    t0 = sbi * 1024
                    st2 = ExitStack()
                    st2.__enter__()
                    rw, R_rw = k.sb("f_rw", [128, KC, NE], F32, st2)
                    rb, R_rb = k.sb("f_rb", [128, 256], F32, st2)
                    bdn, R_bdn = k.sb("f_bdn", [32, D], F32, st2)
                    L, R_L = k.sb("f_L", [128, 256], F32, st2)
                    Gm, R_Gm = k.sb("f_G", [128, 256], F32, st2)
                    m8, R_m8 = k.sb("f_m8", [128, 16], F32, st2)
                    e32, R_e32 = k.sb("f_e32", [128, 64], F32, st2)
                    k.dma(sp, rw[:], router_w[l].rearrange("(k p) e -> p k e", p=128), [R_in], [R_rw], R_rw)
                    k.dma(sp, rb[:], rb_row[l], [R_in], [R_rb], R_rb)
                    k.dma(sp, bdn[:], b_down[l], [R_in], [R_bdn], R_bdn)
                    norm_mod(st2, l, sbi, a2v, SH2, h2, R_h2, h_f32=yacc, R_hf=R_yacc)
                    ps, R_ps = k.nextps()
                    for tt in range(8):
                        k.mm(ps[:, tt * 32:(tt + 1) * 32],
                             [(yacc[:, kk, tt * 128:(tt + 1) * 128], rw[:, kk, :]) for kk in range(KC)],
                             [R_yacc, R_rw], [R_ps])
                    k.op(dve, lambda: V.tensor_tensor(out=L[:], in0=ps[:, 0:256], in1=rb[:], op=ALU.add),
                         [R_ps, R_rb], [R_L])
                    for tt in range(8):
                        ls = slice(tt * 32, (tt + 1) * 32)
                        k.op(dve, lambda: V.max(out=m8[:, 0:8], in_=L[:, ls]), [R_L], [R_m8])
                        k.op(dve, lambda: V.tensor_scalar(out=m8[:, 8:9], in0=m8[:, 0:1], scalar1=-1.0, scalar2=None,
                                                          op0=ALU.mult), [R_m8], [R_m8])
                        k.op(act, lambda: A.activation(out=e32[:, 0:32], in_=L[:, ls], func=AF.Exp,
                                                       bias=m8[:, 8:9], scale=1.0), [R_L, R_m8], [R_e32])
                        k.op(dve, lambda: V.tensor_scalar(out=e32[:, 32:64], in0=L[:, ls], scalar1=m8[:, 3:4],
                                                          scalar2=None, op0=ALU.is_ge), [R_L, R_m8], [R_e32])
                        k.op(dve, lambda: V.tensor_tensor(out=e32[:, 0:32], in0=e32[:, 0:32], in1=e32[:, 32:64],
                                                          op=ALU.mult), [R_e32], [R_e32])
                        k.op(dve, lambda: V.reduce_sum(out=m8[:, 9:10], in_=e32[:, 0:32], axis=AX.X), [R_e32], [R_m8])
                        k.op(dve, lambda: V.reciprocal(out=m8[:, 10:11], in_=m8[:, 9:10]), [R_m8], [R_m8])
                        k.op(dve, lambda: V.tensor_scalar(out=Gm[:, ls], in0=e32[:, 0:32], scalar1=m8[:, 10:11],
                                                          scalar2=None, op0=ALU.mult), [R_e32, R_m8], [R_Gm])
                    for half in range(2):
                        ps, R_ps = k.nextps()
                        for t4 in range(4):
                            tt = half * 4 + t4
                            k.tr(ps[0:32, t4 * 128:(t4 + 1) * 128], Gm[:, tt * 32:(tt + 1) * 32], ident[:],
                                 [R_Gm, R_ident], [R_ps])
                        k.op(dve, lambda: V.tensor_copy(out=GT[:, half * 512:(half + 1) * 512], in_=ps[0:32, :]),
                             [R_ps], [R_GT])
                    for m in range(KC):
                        for half in range(2):
                            hs = slice(half * 512, (half + 1) * 512)
                            ps, R_ps = k.nextps()
                            k.mm(ps[:], [(bdn[:, m * 128:(m + 1) * 128], GT[:, hs])], [R_bdn, R_GT], [R_ps])
                            k.op(act, lambda: A.copy(out=yacc[:, m, hs], in_=ps[:]), [R_ps], [R_yacc])
                    k.barrier()
                    st2.__exit__(None, None, None)
                    st3 = ExitStack()
                    st3.__enter__()
                    actT, R_actT = k.sb("f_act", [128, 6, 1024], BF16, st3)
                    wu = [k.sb(f"f_wu{i}", [128, KC, 256], BF16, st3) for i in range(3)]
                    wd = [k.sb(f"f_wd{i}", [128, 6, 1024], BF16, st3) for i in range(2)]
                    tg_ = [k.sb(f"f_tg{i}", [128, 512], F32, st3) for i in range(2)]
                    ts_ = [k.sb(f"f_ts{i}", [128, 512], F32, st3) for i in range(2)]
                    tl_ = [k.sb(f"f_tl{i}", [128, 512], F32, st3) for i in range(2)]
                    jobs = []
                    for e in range(NE):
                        jobs += [("u", e, c) for c in range(6)] + [("d", e, mh) for mh in range(2)]
                    cnts = {"u": 0, "d": 0}

                    def issue_w(j, job):
                        kind, e, c = job
                        if kind == "u":
                            wut, R_wu = wu[cnts["u"] % 3]
                            cnts["u"] += 1
                            k.dma(pool, wut[:], w_up_r[l, e, c].rearrange("p (k c) -> p k c", c=256),
                                  [R_in], [R_wu], R_wu)
                            return wut, R_wu
                        wdt, R_wd = wd[cnts["d"] % 2]
                        cnts["d"] += 1
                        k.dma(pool, wdt[:], w_down_r[l, e, c].rearrange("p (k c) -> p k c", c=1024),
                              [R_in], [R_wd], R_wd)
                        return wdt, R_wd
                    getw = prefetcher(jobs, issue_w, 2)
                    for e in range(NE):
                        st_, R_sel = sel[e % 2]
                        k.op(dve, lambda: V.tensor_scalar(out=st_[:], in0=iota32[:], scalar1=float(e), scalar2=None,
                                                          op0=ALU.is_equal), [R_iota], [R_sel])
                        gb_, R_gb = gbc[e % 2]
                        for half in range(2):
                            hs = slice(half * 512, (half + 1) * 512)
                            ps, R_ps = k.nextps()
                            k.mm(ps[:], [(st_[:], GT[:, hs])], [R_sel, R_GT], [R_ps])
                            k.op(act, lambda: A.copy(out=gb_[:, hs], in_=ps[:]), [R_ps], [R_gb])
                        for c in range(6):
                            wut, R_wu = getw(e * 8 + c)
                            for half in range(2):
                                hs = slice(half * 512, (half + 1) * 512)
                                psg, R_psg = k.nextps()
                                k.mm(psg[:], [(wut[:, kk, 0:128], h2[:, kk, hs]) for kk in range(KC)],
                                     [R_wu, R_h2], [R_psg])
                                psl, R_psl = k.nextps()
                                k.mm(psl[:], [(wut[:, kk, 128:256], h2[:, kk, hs]) for kk in range(KC)],
                                     [R_wu, R_h2], [R_psl])
                                tg, R_tg = tg_[ecnt % 2]
                                ts, R_ts = ts_[ecnt % 2]
                                tl, R_tl = tl_[ecnt % 2]
                                ecnt += 1
                                bgc = bup[:, e * 12 + c:e * 12 + c + 1]
                                blc = bup[:, e * 12 + 6 + c:e * 12 + 6 + c + 1]
                                k.op(dve, lambda: V.tensor_scalar(out=tg[:], in0=psg[:], scalar1=bgc, scalar2=7.0,
                                                                  op0=ALU.add, op1=ALU.min), [R_psg, R_bup], [R_tg])
                                k.op(act, lambda: A.activation(out=ts[:], in_=tg[:], func=AF.Sigmoid, scale=1.702),
                                     [R_tg], [R_ts])
                                k.op(dve, lambda: V.tensor_scalar(out=tl[:], in0=psl[:], scalar1=blc, scalar2=7.0,
                                                                  op0=ALU.add, op1=ALU.min), [R_psl, R_bup], [R_tl])
                                k.op(dve, lambda: V.tensor_scalar(out=tl[:], in0=tl[:], scalar1=-7.0, scalar2=1.0,
                                                                  op0=ALU.max, op1=ALU.add), [R_tl], [R_tl])
                                k.op(pool, lambda: G.tensor_tensor(out=tg[:], in0=tg[:], in1=ts[:], op=ALU.mult),
                                     [R_tg, R_ts], [R_tg])
                                k.op(pool, lambda: G.tensor_tensor(out=tl[:], in0=tl[:], in1=gb_[:, hs], op=ALU.mult),
                                     [R_tl, R_gb], [R_tl])
                                k.op(dve, lambda: V.tensor_tensor(out=actT[:, c, hs], in0=tg[:], in1=tl[:],
                                                                  op=ALU.mult), [R_tg, R_tl], [R_actT])
                        for mh in range(2):
                            wdt, R_wd = getw(e * 8 + 6 + mh)
                            for m8_ in range(8):
                                m = mh * 8 + m8_
                                for half in range(2):
                                    hs = slice(half * 512, (half + 1) * 512)
                                    ps, R_ps = k.nextps()
                                    k.mm(ps[:], [(wdt[:, c, m8_ * 128:(m8_ + 1) * 128], actT[:, c, hs])
                                                 for c in range(6)], [R_wd, R_actT], [R_ps])
                                    k.op(dve, lambda: V.tensor_tensor(out=yacc[:, m, hs], in0=yacc[:, m, hs],
                                                                      in1=ps[:], op=ALU.add), [R_ps, R_yacc], [R_yacc])
                    for m in range(KC):
                        xot, R_xo = gbc[m % 2]
                        k.dma(sp, xot[:], xT[m * 128:(m + 1) * 128, t0:t0 + 1024], [R_xTs[m]], [R_xo], R_xo)
                        k.op(dve, lambda: V.scalar_tensor_tensor(out=xot[:], in0=yacc[:, m, :],
                                                                 scalar=modv[:, l, GT2 + m:GT2 + m + 1], in1=xot[:],
                                                                 op0=ALU.mult, op1=ALU.add),
                             [R_yacc, R_mod, R_xo], [R_xo])
                        k.dma(sp, xT[m * 128:(m + 1) * 128, t0:t0 + 1024], xot[:], [R_xo], [R_xTs[m]], R_xo)
                    k.barrier()
                    st3.__exit__(None, None, None)

        with ExitStack() as st:
            gf, R_gf = k.sb("z_gf", [128, KC], F32, st)
            xb, R_xb = k.sb("z_x", [128, KC, 512], F32, st)
            sq = [k.sb(f"z_sq{i}", [128, 512], F32, st) for i in range(2)]
            rs, R_rs = k.sb("z_rs", [128, 512], F32, st)
            ob = [k.sb(f"z_o{i}", [128, 4, D], F32, st) for i in range(2)]
            k.dma(sp, gf[:], gfin_col, [R_in], [R_gf], R_gf)
            ov = out_d.rearrange("(n p) d -> p n d", p=128)
            for b in range(NB):
                k.dma(sp, xb[:], xTv[:, :, b * 512:(b + 1) * 512], R_xTs, [R_xb], R_xb)
                obt, R_ob = ob[b % 2]
                if final:
                    ps, R_ps = k.nextps()
                    for kk in range(KC):
                        sqt, R_sq = sq[kk % 2]
                        k.op(act, lambda: A.activation(out=sqt[:], in_=xb[:, kk, :], func=AF.Square), [R_xb], [R_sq])
                        k.mm1(ps[:], ones_f[:], sqt[:], kk == 0, kk == KC - 1, [R_onesf, R_sq], [R_ps])
                    k.op(dve, lambda: V.tensor_scalar(out=rs[:], in0=ps[:], scalar1=1.0 / D, scalar2=EPS,
                                                      op0=ALU.mult, op1=ALU.add), [R_ps], [R_rs])
                    k.op(act, lambda: A.activation(out=rs[:], in_=rs[:], func=AF.Sqrt), [R_rs], [R_rs])
                    k.op(dve, lambda: V.reciprocal(out=rs[:], in_=rs[:]), [R_rs], [R_rs])
                    for kk in range(KC):
                        k.op(dve, lambda: V.scalar_tensor_tensor(out=xb[:, kk, :], in0=xb[:, kk, :],
                                                                 scalar=gf[:, kk:kk + 1], in1=rs[:],
                                                                 op0=ALU.mult, op1=ALU.mult),
                             [R_xb, R_gf, R_rs], [R_xb])
                for tt in range(4):
                    for k4 in range(4):
                        ps, R_ps = k.nextps()
                        for q4 in range(4):
                            kk = k4 * 4 + q4
                            k.tr(ps[:, q4 * 128:(q4 + 1) * 128], xb[:, kk, tt * 128:(tt + 1) * 128], ident[:],
                                 [R_xb, R_ident], [R_ps])
                        if k4 % 2 == 0:
                            k.op(dve, lambda: V.tensor_copy(out=obt[:, tt, k4 * 512:(k4 + 1) * 512], in_=ps[:]),
                                 [R_ps], [R_ob])
                        else:
                            k.op(act, lambda: A.copy(out=obt[:, tt, k4 * 512:(k4 + 1) * 512], in_=ps[:]),
                                 [R_ps], [R_ob])
                k.dma(sp, ov[:, b * 4:(b + 1) * 4, :], obt[:], [R_ob], [R_out], R_ob)
            k.barrier()
    return nc


def _col(v):
    return np.ascontiguousarray(v.reshape(-1, 128).T)


def prep_shared(inp):
    f = np.float32
    L = DEPTH
    sh = {}
    sh["w_ada"] = inp["w_ada"]
    sh["b_ada_col"] = np.stack([_col(inp["b_ada"][l]) for l in range(L)])
    sh["gmix_col"] = np.stack([_col(inp["g_norm_mix"][l]) for l in range(L)])
    wi = inp["w_in"]
    wic = np.concatenate([wi[:, :, 0:O_F], wi[:, :, O_GU:IN_DIM]], axis=2)
    sh["w_in_r"] = wic.reshape(L, KC, 128, 11, 512).transpose(0, 3, 2, 1, 4).reshape(L, 11, 128, KC * 512)
    sh["wf_r"] = wi[:, :, O_F:O_F + 8].reshape(L, KC, 128, 8).transpose(0, 2, 1, 3).reshape(L, 128, KC * 8)
    sh["pool_w"] = inp["pool_w"]
    sh["pscale_col"] = np.stack([_col(inp["pool_scale"][l]) for l in range(L)])
    sh["convw_col"] = np.ascontiguousarray(
        inp["conv_w"].reshape(L, 31, 4, 128).transpose(0, 3, 2, 1))
    sh["convb_col"] = np.stack([_col(inp["conv_b"][l]) for l in range(L)])
    sh["clng_col"] = np.stack([_col(inp["conv_ln_g"][l]) for l in range(L)])
    sh["clnb_col"] = np.stack([_col(inp["conv_ln_b"][l]) for l in range(L)])
    sh["fgb_row"] = np.ascontiguousarray(np.broadcast_to(inp["fgate_b"][:, None, :], (L, 128, 8)))
    sh["glng_row"] = np.ascontiguousarray(np.broadcast_to(inp["gmlp_ln_g"][:, None, :], (L, 128, 512)))
    sh["gmlp_ws"] = inp["gmlp_ws"]
    sh["gbs_row"] = np.ascontiguousarray(
        np.broadcast_to(inp["gmlp_bs"].reshape(L, 1, 512), (L, 128, 512)))
    sh["w_gate_r"] = inp["w_gate"].reshape(L, KC, 128, 4, KC, 128).transpose(0, 4, 2, 3, 1, 5).reshape(
        L, KC, 128, 4 * KC * 128)
    sh["bgate_col"] = np.stack([_col(inp["b_gate"][l]) for l in range(L)])
    sh["w_branch_r"] = inp["w_branch"].reshape(L, 20, 128, KC, 128).transpose(0, 3, 2, 1, 4).reshape(
        L, KC, 128, 20 * 128)
    sh["w_o_r"] = inp["w_o"].reshape(L, KC, 128, KC, 128).transpose(0, 3, 2, 1, 4).reshape(L, KC, 128, KC * 128)
    sh["gffn_col"] = np.stack([_col(inp["g_norm_ffn"][l]) for l in range(L)])
    sh["router_w"] = inp["router_w"]
    sh["rb_row"] = np.ascontiguousarray(
        np.broadcast_to(np.tile(inp["router_b"], (1, 8))[:, None, :], (L, 128, 256)))
    sh["w_up_r"] = inp["w_up"].reshape(L, NE, KC, 128, 2, 6, 128).transpose(0, 1, 5, 3, 2, 4, 6).reshape(
        L, NE, 6, 128, KC * 256)
    sh["bup_col"] = np.ascontiguousarray(
        inp["b_up"].reshape(L, NE, 12, 128).transpose(0, 3, 1, 2).reshape(L, 128, NE * 12))
    sh["w_down_r"] = inp["w_down"].reshape(L, NE, 6, 128, 2, 1024).transpose(0, 1, 4, 3, 2, 5).reshape(
        L, NE, 2, 128, 6 * 1024)
    sh["b_down"] = inp["b_down"]
    sh["gfin_col"] = _col(inp["g_final"])
    sh["c_ident"] = np.eye(128, dtype=f)
    sh["c_tril"] = np.triu(np.ones((128, 128), f))
    sh["c_trilT"] = np.tril(np.ones((128, 128), f))
    t = np.arange(512)
    ic = np.stack([1.0 / np.minimum(t + 1, w) for w in (2, 4, 8, 16)]).astype(f)
    sh["c_invcnt"] = np.ascontiguousarray(np.broadcast_to(ic[None], (128, 4, 512)))
    sh["c_iota32"] = np.ascontiguousarray(np.broadcast_to(np.arange(32, dtype=f)[:, None], (32, 128)))
    return {k_: np.ascontiguousarray(v, dtype=f) for k_, v in sh.items()}


SPLIT = True


def kernel(**inputs):
    inp = {k_: np.asarray(v) for k_, v in inputs.items()}
    x = inp["x"].astype(np.float32)
    c = inp["c"].astype(np.float32)
    B = x.shape[0]
    sh = prep_shared(inp)
    in_maps = []
    if SPLIT:
        T = SEQ // 2
        nc = build(T, split=True)
        ic2 = np.ascontiguousarray(np.broadcast_to(
            np.array([1.0 / w for w in (2, 4, 8, 16)], np.float32)[None, :, None], (128, 4, 512)))
        for core in range(8):
            b, half = core // 2, core % 2
            m = dict(sh)
            m["x"] = np.ascontiguousarray(x[b, half * T:(half + 1) * T])
            m["c_col"] = _col(c[b])
            m["flag_col"] = np.full((128, 1), float(half), np.float32)
            m["pmb_col"] = np.full((128, 1), 0.0 if half else -30000.0, np.float32)
            if half:
                m["c_invcnt"] = ic2
            in_maps.append(m)
        res = run_bass_kernel_spmd(nc, in_maps, core_ids=list(range(8)))
        out = np.stack([np.concatenate([res.results[2 * b]["out"], res.results[2 * b + 1]["out"]], axis=0)
                        for b in range(B)], axis=0)
        return out.astype(np.float32)
    nc = build(SEQ)
    for core in range(8):
        b = core % B
        m = dict(sh)
        m["x"] = np.ascontiguousarray(x[b])
        m["c_col"] = _col(c[b])
        m["flag_col"] = np.zeros((128, 1), np.float32)
        m["pmb_col"] = np.zeros((128, 1), np.float32)
        in_maps.append(m)
    res = run_bass_kernel_spmd(nc, in_maps, core_ids=list(range(8)))
    out = np.stack([res.results[b]["out"] for b in range(B)], axis=0)
    return out.astype(np.float32)
```

```python
from contextlib import ExitStack
import numpy as np
import concourse.bass as bass
import concourse.mybir as mybir
from concourse.bass_utils import run_bass_kernel_spmd

F32 = mybir.dt.float32
BF16 = mybir.dt.bfloat16
AF = mybir.ActivationFunctionType
ALU = mybir.AluOpType
AX = mybir.AxisListType

D = 2048
KC = 16
SEQ = 4096
DEPTH = 4
NE = 32
FF = 768
IN_DIM = 5640
EPS = 1e-5
O_POOL, O_CA, O_CB, O_Q, O_K, O_V, O_F, O_GU, O_GV = 0, 512, 1024, 1536, 2560, 3584, 4608, 4616, 5128


class Res:
    __slots__ = ("w", "r", "dsem", "dcnt", "name")

    def __init__(self, name=""):
        self.w = None
        self.r = {}
        self.dsem = None
        self.dcnt = 0
        self.name = name


class Eng:
    def __init__(self, h, sem, is_pe=False):
        self.h = h
        self.sem = sem
        self.n = 0
        self.known = {}
        self.is_pe = is_pe


class K:
    def __init__(self, nc, es):
        self.nc = nc
        self.es = es
        sem = lambda n: es.enter_context(nc.semaphore(n))
        self.pe = Eng(nc.tensor, sem("q_pe"), True)
        self.act = Eng(nc.scalar, sem("q_act"))
        self.dve = Eng(nc.vector, sem("q_dve"))
        self.pool = Eng(nc.gpsimd, sem("q_pool"))
        self.sp = Eng(nc.sync, sem("q_sp"))
        self.engs = [self.pe, self.act, self.dve, self.pool, self.sp]
        self.dres = []
        self.nsem = 5
        self.sempool = []
        self.ps = []
        self.psr = []
        for i in range(8):
            self.ps.append(es.enter_context(nc.psum_tensor(f"psb{i}", [128, 512], F32)))
            self.psr.append(Res(f"ps{i}"))
        self.psi = 0

    def sb(self, name, shape, dt, stack=None):
        self.uid = getattr(self, "uid", 0) + 1
        t = (stack or self.es).enter_context(self.nc.sbuf_tensor(f"{name}_{self.uid}", shape, dt))
        return t, Res(name)

    def nextps(self):
        i = self.psi
        self.psi = (i + 1) % 8
        return self.ps[i], self.psr[i]

    def _need(self, q, tok):
        sem, val = tok
        if q.known.get(id(sem), 0) >= val:
            return
        q.h.wait_ge(sem, val)
        q.known[id(sem)] = val

    def _deps(self, q, R, W):
        toks = []
        for r in R:
            if r.w is not None:
                toks.append(r.w)
        for w in W:
            if w.w is not None:
                toks.append(w.w)
            toks.extend(w.r.values())
        for t in toks:
            if q.is_pe and t[0] is q.sem:
                continue
            self._need(q, t)

    def _mark(self, tok, R, W):
        for r in R:
            r.r[id(tok[0])] = tok
        for w in W:
            w.w = tok
            w.r = {}

    def op(self, q, fn, R=(), W=()):
        self._deps(q, R, W)
        ins = fn()
        q.n += 1
        ins.then_inc(q.sem, 1)
        self._mark((q.sem, q.n), R, W)
        return ins

    def mm(self, out_ap, pairs, R, W, fp32=False):
        q = self.pe
        self._deps(q, R, W)
        n = len(pairs)
        ins = None
        for i, (l, r) in enumerate(pairs):
            ins = self.nc.tensor.matmul(out_ap, lhsT=l, rhs=r, start=(i == 0), stop=(i == n - 1))
        q.n += 1
        ins.then_inc(q.sem, 1)
        self._mark((q.sem, q.n), R, W)

    def mm1(self, out_ap, l, r, start, stop, R, W):
        q = self.pe
        self._deps(q, R, W)
        ins = self.nc.tensor.matmul(out_ap, lhsT=l, rhs=r, start=start, stop=stop)
        q.n += 1
        ins.then_inc(q.sem, 1)
        self._mark((q.sem, q.n), R, W)

    def tr(self, out_ap, in_ap, ident_ap, R, W):
        q = self.pe
        self._deps(q, R, W)
        ins = self.nc.tensor.transpose(out=out_ap, in_=in_ap, identity=ident_ap)
        q.n += 1
        ins.then_inc(q.sem, 1)
        self._mark((q.sem, q.n), R, W)

    def dma(self, q, out_ap, in_ap, R, W, sres):
        if sres.dsem is None:
            if self.sempool:
                sres.dsem, sres.dcnt = self.sempool.pop()
            else:
                sres.dsem = self.es.enter_context(self.nc.semaphore(f"dsem{self.nsem}"))
                sres.dcnt = 0
                self.nsem += 1
            self.dres.append(sres)
        self._deps(q, R, W)
        ins = q.h.dma_start(out=out_ap, in_=in_ap)
        sres.dcnt += 16
        ins.then_inc(sres.dsem, 16)
        self._mark((sres.dsem, sres.dcnt), R, W)

    def barrier(self):
        toks = [(e.sem, e.n) for e in self.engs if e.n > 0]
        toks += [(r.dsem, r.dcnt) for r in self.dres if r.dcnt > 0]
        for q in self.engs:
            for t in toks:
                if t[0] is q.sem:
                    continue
                self._need(q, t)
        for r in self.dres:
            self.sempool.append((r.dsem, r.dcnt))
            r.dsem = None
        self.dres = []


def prefetcher(jobs, issue, depth):
    state = {"n": 0, "res": {}}

    def get(i):
        while state["n"] <= min(i + depth, len(jobs) - 1):
            j = state["n"]
            state["res"][j] = issue(j, jobs[j])
            state["n"] += 1
        return state["res"].pop(i)
    return get


def build(T, nlayers=DEPTH, do_moe=True, final=True, LW=DEPTH, split=False):
    NT = T // 128
    NB = T // 512
    NSB = T // 1024
    nc = bass.Bass("TRN2", target_bir_lowering=False)
    din = lambda n, s, dt=F32: nc.dram_tensor(n, s, dt, kind="ExternalInput").ap()
    x_in = din("x", [T, D])
    c_col = din("c_col", [128, KC])
    w_ada = din("w_ada", [LW, D, 6 * D])
    b_ada_col = din("b_ada_col", [LW, 128, 96])
    gmix_col = din("gmix_col", [LW, 128, KC])
    w_in_r = din("w_in_r", [LW, 11, 128, KC * 512])
    wf_r = din("wf_r", [LW, 128, KC * 8])
    pool_w = din("pool_w", [LW, 4, 128, 128])
    pscale_col = din("pscale_col", [LW, 128, 4])
    convw_col = din("convw_col", [LW, 128, 4, 31])
    convb_col = din("convb_col", [LW, 128, 4])
    clng_col = din("clng_col", [LW, 128, 4])
    clnb_col = din("clnb_col", [LW, 128, 4])
    fgb_row = din("fgb_row", [LW, 128, 8])
    glng_row = din("glng_row", [LW, 128, 512])
    gmlp_ws = din("gmlp_ws", [LW, 4, 128, 128])
    gbs_row = din("gbs_row", [LW, 128, 512])
    w_gate_r = din("w_gate_r", [LW, KC, 128, 4 * KC * 128])
    bgate_col = din("bgate_col", [LW, 128, 64])
    w_branch_r = din("w_branch_r", [LW, KC, 128, 20 * 128])
    w_o_r = din("w_o_r", [LW, KC, 128, KC * 128])
    gffn_col = din("gffn_col", [LW, 128, KC])
    router_w = din("router_w", [LW, D, NE])
    rb_row = din("rb_row", [LW, 128, 256])
    w_up_r = din("w_up_r", [LW, NE, 6, 128, KC * 256])
    bup_col = din("bup_col", [LW, 128, NE * 12])
    w_down_r = din("w_down_r", [LW, NE, 2, 128, 6 * 1024])
    b_down = din("b_down", [LW, NE, D])
    gfin_col = din("gfin_col", [128, KC])
    c_ident = din("c_ident", [128, 128])
    c_tril = din("c_tril", [128, 128])
    c_trilT = din("c_trilT", [128, 128])
    c_invcnt = din("c_invcnt", [128, 4, 512])
    c_iota32 = din("c_iota32", [32, 128])
    flag_col = din("flag_col", [128, 1])
    pmb_col = din("pmb_col", [128, 1])
    out_d = nc.dram_tensor("out", [T, D], F32, kind="ExternalOutput").ap()

    dsc = lambda n, s, dt: nc.dram_tensor(n, s, dt).ap()
    xT = dsc("s_xT", [D, T], F32)
    hT_d = dsc("s_hT", [D, T], BF16)
    zp_d = dsc("s_zp", [512, T], F32)
    glu_d = dsc("s_glu", [512, T], BF16)
    qT_d = dsc("s_qT", [1024, T], BF16)
    CH = T // 2
    xkv = [[dsc(f"s_xkv{i}_{q}", [CH, 1024], BF16) for q in range(4)] for i in range(2)]
    kTa_all = [xk[0].rearrange("(a b) c -> a (b c)", b=T // 1024) for xk in xkv]
    kTb_all = [xk[1].rearrange("(a b) c -> a (b c)", b=T // 1024) for xk in xkv]
    va_all = [xk[2] for xk in xkv]
    vb_all = [xk[3] for xk in xkv]
    if split:
        gkv = [[dsc(f"s_gkv{i}_{q}", [2 * CH, 1024], BF16) for q in range(4)] for i in range(2)]
        xs_d = [dsc(f"s_xs{i}", [128, 320], F32) for i in range(2)]
        gs_d = [dsc(f"s_gs{i}", [256, 320], F32) for i in range(2)]
    R_gkv, R_gs, R_xsd = Res("gkv"), Res("gs"), Res("xsd")
    uT_d = dsc("s_uT", [512, T], BF16)
    gv_d = dsc("s_gv", [T, 512], BF16)
    yT_d = dsc("s_yT", [2560, T], BF16)
    R_hT, R_zp, R_glu, R_q, R_k, R_v, R_u, R_gv, R_y = [Res(n) for n in "hT zp glu q k v u gv y".split()]
    R_xTs = [Res(f"xT{i}") for i in range(KC)]
    R_in = Res("inputs")
    R_out = Res("out")
    xTv = xT.rearrange("(k p) t -> p k t", p=128)
    hTv = hT_d.rearrange("(k p) t -> p k t", p=128)

    es = ExitStack()
    with es:
        k = K(nc, es)
        pe, act, dve, pool, sp = k.pe, k.act, k.dve, k.pool, k.sp
        V, A, G = nc.vector, nc.scalar, nc.gpsimd

        ident, R_ident = k.sb("ident", [128, 128], F32)
        identb, R_identb = k.sb("identb", [128, 128], BF16)
        tril_b, R_trilb = k.sb("tril_b", [128, 128], BF16)
        tril_f, R_trilf = k.sb("tril_f", [128, 128], F32)
        trilT_f, R_trilT = k.sb("trilT_f", [128, 128], F32)
        ones_f, R_onesf = k.sb("ones_f", [128, 128], F32)
        ones_b, R_onesb = k.sb("ones_b", [128, 128], BF16)
        iota32, R_iota = k.sb("iota32", [32, 128], F32)
        ccol, R_ccol = k.sb("ccol", [128, KC], F32)
        modv, R_mod = k.sb("modv", [128, DEPTH, 96], F32)
        a1v, R_a1 = k.sb("a1v", [128, DEPTH, KC], F32)
        a2v, R_a2 = k.sb("a2v", [128, DEPTH, KC], F32)
        lf_sb, R_lf = k.sb("lf_sb", [128, NT, 8], F32)
        within, R_wi = k.sb("a_within", [128, NT, 8], F32)
        tot, R_tot = k.sb("a_tot", [128, NT, 8], F32)
        pinc, R_pinc = k.sb("a_pinc", [128, 8, NT], F32)
        negF, R_negF = k.sb("a_negF", [128, NT, 8], F32)
        flagc, R_flag = k.sb("flagc", [128, 1], F32)
        pmbc, R_pmb = k.sb("pmbc", [128, 1], F32)
        k.dma(sp, flagc[:], flag_col, [R_in], [R_flag], R_flag)
        k.dma(sp, pmbc[:], pmb_col, [R_in], [R_pmb], R_pmb)
        ccn = [0]

        def coll(in_ap, out_ap, R, W):
            csem = es.enter_context(nc.semaphore(f"ccsem{ccn[0]}"))
            ccn[0] += 1
            k._deps(pool, R, W)
            ins = G.collective_compute("AllGather", ALU.bypass, replica_groups=[[0, 1], [2, 3], [4, 5], [6, 7]],
                                       ins=[in_ap], outs=[out_ap])
            ins.then_inc(csem)
            k._mark((csem, 1), R, W)
        k.dma(sp, ident[:], c_ident, [R_in], [R_ident], R_ident)
        k.dma(pool, identb[:], c_ident, [R_in], [R_identb], R_identb)
        k.dma(pool, tril_b[:], c_tril, [R_in], [R_trilb], R_trilb)
        k.dma(sp, tril_f[:], c_tril, [R_in], [R_trilf], R_trilf)
        k.dma(sp, trilT_f[:], c_trilT, [R_in], [R_trilT], R_trilT)
        k.dma(sp, iota32[:], c_iota32, [R_in], [R_iota], R_iota)
        k.dma(sp, ccol[:], c_col, [R_in], [R_ccol], R_ccol)
        k.op(dve, lambda: V.memset(ones_f[:], 1.0), [], [R_onesf])
        k.op(dve, lambda: V.memset(ones_b[:], 1.0), [], [R_onesb])

        with ExitStack() as st:
            cact, R_cact = k.sb("cact", [128, KC], F32, st)
            wa = [k.sb(f"wa{i}", [128, KC, 512], F32, st) for i in range(2)]
            tmpA, R_tmpA = k.sb("tmpA", [128, 96], F32, st)
            k.op(act, lambda: A.activation(out=cact[:], in_=ccol[:], func=AF.Silu), [R_ccol], [R_cact])
            cnt = 0
            for l in range(nlayers):
                wv = w_ada[l].rearrange("(k p) c -> p k c", p=128)
                ps, R_ps = k.nextps()
                for blk in range(24):
                    wt, R_wt = wa[cnt % 2]
                    cnt += 1
                    k.dma(sp, wt[:], wv[:, :, blk * 512:(blk + 1) * 512], [R_in], [R_wt], R_wt)
                    for j in range(4):
                        col = blk * 4 + j
                        k.mm(ps[:, col:col + 1],
                             [(wt[:, kk, j * 128:(j + 1) * 128], cact[:, kk:kk + 1]) for kk in range(KC)],
                             [R_wt, R_cact], [R_ps])
                k.dma(sp, tmpA[:], b_ada_col[l], [R_in], [R_tmpA], R_tmpA)
                k.op(dve, lambda: V.tensor_tensor(out=modv[:, l, :], in0=ps[:, 0:96], in1=tmpA[:], op=ALU.add),
                     [R_ps, R_tmpA], [R_mod])
                k.dma(sp, tmpA[:, 0:16], gmix_col[l], [R_in], [R_tmpA], R_tmpA)
                k.dma(sp, tmpA[:, 16:32], gffn_col[l], [R_in], [R_tmpA], R_tmpA)
                k.op(dve, lambda: V.scalar_tensor_tensor(out=a1v[:, l, :], in0=modv[:, l, 16:32], scalar=1.0,
                                                         in1=tmpA[:, 0:16], op0=ALU.add, op1=ALU.mult),
                     [R_mod, R_tmpA], [R_a1])
                k.op(dve, lambda: V.scalar_tensor_tensor(out=a2v[:, l, :], in0=modv[:, l, 64:80], scalar=1.0,
                                                         in1=tmpA[:, 16:32], op0=ALU.add, op1=ALU.mult),
                     [R_mod, R_tmpA], [R_a2])
            k.barrier()
        SH1, GT1, SH2, GT2 = 0, 32, 48, 80

        with ExitStack() as st:
            xin = [k.sb(f"xin{i}", [128, 4, D], F32, st) for i in range(2)]
            xo = [k.sb(f"xo{i}", [128, KC, 512], F32, st) for i in range(2)]
            xv = x_in.rearrange("(n p) d -> p n d", p=128)
            for b in range(NB):
                xi, R_xi = xin[b % 2]
                xot, R_xo = xo[b % 2]
                k.dma(sp, xi[:], xv[:, b * 4:(b + 1) * 4, :], [R_in], [R_xi], R_xi)
                for kk in range(KC):
                    ps, R_ps = k.nextps()
                    for tt in range(4):
                        k.tr(ps[:, tt * 128:(tt + 1) * 128], xi[:, tt, kk * 128:(kk + 1) * 128], ident[:],
                             [R_xi, R_ident], [R_ps])
                    if kk % 2 == 0:
                        k.op(dve, lambda: V.tensor_copy(out=xot[:, kk, :], in_=ps[:]), [R_ps], [R_xo])
                    else:
                        k.op(act, lambda: A.copy(out=xot[:, kk, :], in_=ps[:]), [R_ps], [R_xo])
                k.dma(sp, xTv[:, :, b * 512:(b + 1) * 512], xot[:], [R_xo], R_xTs, R_xo)
            k.barrier()

        def norm_mod(st, l, sbi, avec, shoff, h_bf, R_hbf, h_f32=None, R_hf=None, store_h=False):
            xc = [k.sb(f"nm_x{i}", [128, 512], F32, st) for i in range(3)]
            sq = [k.sb(f"nm_sq{i}", [128, 512], F32, st) for i in range(2)]
            rs, R_rs = k.sb("nm_rs", [128, 512], F32, st)
            tm = [k.sb(f"nm_tm{i}", [128, 512], F32, st) for i in range(2)]
            xcn = 0
            for half in range(2):
                c0 = sbi * 1024 + half * 512
                ps, R_ps = k.nextps()
                for kk in range(KC):
                    xct, R_xc = xc[xcn % 3]
                    xcn += 1
                    k.dma(sp, xct[:], xT[kk * 128:(kk + 1) * 128, c0:c0 + 512], [R_xTs[kk]], [R_xc], R_xc)
                    sqt, R_sq = sq[kk % 2]
                    k.op(act, lambda: A.activation(out=sqt[:], in_=xct[:], func=AF.Square), [R_xc], [R_sq])
                    k.mm1(ps[:], ones_f[:], sqt[:], kk == 0, kk == KC - 1, [R_onesf, R_sq], [R_ps])
                k.op(dve, lambda: V.tensor_scalar(out=rs[:], in0=ps[:], scalar1=1.0 / D, scalar2=EPS,
                                                  op0=ALU.mult, op1=ALU.add), [R_ps], [R_rs])
                k.op(act, lambda: A.activation(out=rs[:], in_=rs[:], func=AF.Sqrt), [R_rs], [R_rs])
                k.op(dve, lambda: V.reciprocal(out=rs[:], in_=rs[:]), [R_rs], [R_rs])
                for kk in range(KC):
                    xct, R_xc = xc[xcn % 3]
                    xcn += 1
                    k.dma(sp, xct[:], xT[kk * 128:(kk + 1) * 128, c0:c0 + 512], [R_xTs[kk]], [R_xc], R_xc)
                    tmt, R_tm = tm[kk % 2]
                    k.op(dve, lambda: V.tensor_tensor(out=tmt[:], in0=xct[:], in1=rs[:], op=ALU.mult),
                         [R_xc, R_rs], [R_tm])
                    k.op(act, lambda: A.activation(out=h_bf[:, kk, half * 512:(half + 1) * 512], in_=tmt[:],
                                                   func=AF.Identity, bias=modv[:, l, shoff + kk:shoff + kk + 1],
                                                   scale=avec[:, l, kk:kk + 1]),
                         [R_tm, R_mod, R_a1, R_a2], [R_hbf])
                    if h_f32 is not None:
                        k.op(act, lambda: A.activation(out=h_f32[:, kk, half * 512:(half + 1) * 512], in_=tmt[:],
                                                       func=AF.Identity, bias=modv[:, l, shoff + kk:shoff + kk + 1],
                                                       scale=avec[:, l, kk:kk + 1]),
                             [R_tm, R_mod, R_a1, R_a2], [R_hf])
            if store_h:
                k.dma(sp, hTv[:, :, sbi * 1024:(sbi + 1) * 1024], h_bf[:], [R_hbf], [R_hT], R_hbf)

        def gelu_from_ps(ps, R_ps, outap, R_out, tA, R_tA, tB, R_tB, width=512):
            k.op(act, lambda: A.activation(out=tA[:, :width], in_=ps[:, :width], func=AF.Square), [R_ps], [R_tA])
            k.op(dve, lambda: V.tensor_scalar(out=tA[:, :width], in0=tA[:, :width], scalar1=0.044715, scalar2=1.0,
                                              op0=ALU.mult, op1=ALU.add), [R_tA], [R_tA])
            k.op(dve, lambda: V.tensor_tensor(out=tA[:, :width], in0=tA[:, :width], in1=ps[:, :width], op=ALU.mult),
                 [R_tA, R_ps], [R_tA])
            k.op(act, lambda: A.activation(out=tB[:, :width], in_=tA[:, :width], func=AF.Sigmoid,
                                           scale=1.5957691216057308), [R_tA], [R_tB])
            k.op(dve, lambda: V.tensor_tensor(out=outap, in0=tB[:, :width], in1=ps[:, :width], op=ALU.mult),
                 [R_tB, R_ps], [R_out])

        for l in range(nlayers):
            kTa, kTb, v_a, v_b = kTa_all[l % 2], kTb_all[l % 2], va_all[l % 2], vb_all[l % 2]
            with ExitStack() as st:
                hb, R_hb = k.sb("c_h", [128, KC, 1024], BF16, st)
                wsl = [k.sb(f"c_w{i}", [128, KC, 512], BF16, st) for i in range(3)]
                wf, R_wf = k.sb("c_wf", [128, KC, 8], BF16, st)
                sig, R_sig = k.sb("c_sig", [128, 4, 1024], F32, st)
                stb = [k.sb(f"c_stb{i}", [128, 1024], BF16, st) for i in range(3)]
                stf = [k.sb(f"c_stf{i}", [128, 1024], F32, st) for i in range(2)]
                tA, R_tA = k.sb("c_tA", [128, 512], F32, st)
                tB, R_tB = k.sb("c_tB", [128, 512], F32, st)
                tC, R_tC = k.sb("c_tC", [128, 512], F32, st)
                sc8, R_sc8 = k.sb("c_sc8", [128, 8], F32, st)
                fgb, R_fgb = k.sb("c_fgb", [128, 8], F32, st)
                glng, R_glng = k.sb("c_glng", [128, 512], F32, st)
                k.dma(sp, fgb[:], fgb_row[l], [R_in], [R_fgb], R_fgb)
                k.dma(sp, glng[:], glng_row[l], [R_in], [R_glng], R_glng)
                wcnt = [0]
                scnt = [0, 0]

                def loadw(c0, width=512):
                    wt, R_wt = wsl[wcnt[0] % 3]
                    wcnt[0] += 1
                    bi = c0 // 512 if c0 < O_F else 9 + (c0 - O_GU) // 512
                    k.dma(pool, wt[:], w_in_r[l, bi].rearrange("p (k c) -> p k c", c=512), [R_in], [R_wt], R_wt)
                    return wt, R_wt

                for sbi in range(NSB):
                    with ExitStack() as st2:
                        norm_mod(st2, l, sbi, a1v, SH1, hb, R_hb, store_h=True)
                        k.barrier()
                    t0 = sbi * 1024
                    fm = [(O_POOL, "pool", zp_d, 0), (O_CB, "cb", None, 0), (O_CA, "ca", glu_d, 0),
                          (O_Q, "cp", qT_d, 0), (O_Q + 512, "cp", qT_d, 512), (O_K, "cp", kTa, 0),
                          (O_K + 512, "cp", kTb, 0), (O_GU, "gelu", uT_d, 0)]
                    rmap = {id(zp_d): R_zp, id(glu_d): R_glu, id(qT_d): R_q, id(kTa): R_k, id(kTb): R_k, id(uT_d): R_u}
                    cjobs = [f_[0] for f_ in fm] + [O_V, O_V + 512, O_GV]
                    getc = prefetcher(cjobs, lambda j, c0_: loadw(c0_), 2)
                    for ji, (c0, kind, dst, roff) in enumerate(fm):
                        wt, R_wt = getc(ji)
                        for m in range(4):
                            if kind == "pool":
                                so, R_so = stf[scnt[1] % 2]
                                scnt[1] += 1
                            elif kind != "cb":
                                so, R_so = stb[scnt[0] % 3]
                                scnt[0] += 1
                            for half in range(2):
                                ps, R_ps = k.nextps()
                                k.mm(ps[:], [(wt[:, kk, m * 128:(m + 1) * 128], hb[:, kk, half * 512:(half + 1) * 512])
                                             for kk in range(KC)], [R_wt, R_hb], [R_ps])
                                hs = slice(half * 512, (half + 1) * 512)
                                if kind == "pool":
                                    k.op(act, lambda: A.copy(out=so[:, hs], in_=ps[:]), [R_ps], [R_so])
                                elif kind == "cb":
                                    k.op(act, lambda: A.activation(out=sig[:, m, hs], in_=ps[:], func=AF.Sigmoid),
                                         [R_ps], [R_sig])
                                elif kind == "ca":
                                    k.op(dve, lambda: V.tensor_tensor(out=so[:, hs], in0=ps[:], in1=sig[:, m, hs],
                                                                      op=ALU.mult), [R_ps, R_sig], [R_so])
                                elif kind == "cp":
                                    if half == 0:
                                        k.op(act, lambda: A.copy(out=so[:, hs], in_=ps[:]), [R_ps], [R_so])
                                    else:
                                        k.op(dve, lambda: V.tensor_copy(out=so[:, hs], in_=ps[:]), [R_ps], [R_so])
                                elif kind == "gelu":
                                    gelu_from_ps(ps, R_ps, so[:, hs], R_so, tA, R_tA, tB, R_tB)
                            if kind != "cb":
                                r0 = roff + m * 128
                                k.dma(sp, dst[r0:r0 + 128, t0:t0 + 1024], so[:], [R_so], [rmap[id(dst)]], R_so)
                    for ji, (c0, kind, coff) in enumerate([(O_V, "v", 0), (O_V + 512, "v", 512), (O_GV, "gv", 0)]):
                        wt, R_wt = getc(len(fm) + ji)
                        for tt in range(8):
                            ps, R_ps = k.nextps()
                            k.mm(ps[:], [(hb[:, kk, tt * 128:(tt + 1) * 128], wt[:, kk, :]) for kk in range(KC)],
                                 [R_wt, R_hb], [R_ps])
                            so, R_so = stb[scnt[0] % 3]
                            scnt[0] += 1
                            r0 = t0 + tt * 128
                            if kind == "v":
                                k.op(act, lambda: A.copy(out=so[:, 0:512], in_=ps[:]), [R_ps], [R_so])
                                vdst, rr = (v_a, r0) if r0 < CH else (v_b, r0 - CH)
                                k.dma(sp, vdst[rr:rr + 128, coff:coff + 512], so[:, 0:512], [R_so], [R_v], R_so)
                            else:
                                gelu_from_ps(ps, R_ps, tC[:], R_tC, tA, R_tA, tB, R_tB)
                                k.op(dve, lambda: V.memset(sc8[:, 0:1], 0.0), [], [R_sc8])
                                k.op(act, lambda: A.activation(out=tA[:], in_=tC[:], func=AF.Square,
                                                               accum_out=sc8[:, 0:1]), [R_tC], [R_tA, R_sc8])
                                k.op(dve, lambda: V.tensor_scalar(out=sc8[:, 1:2], in0=sc8[:, 0:1], scalar1=1.0 / 512,
                                                                  scalar2=EPS, op0=ALU.mult, op1=ALU.add),
                                     [R_sc8], [R_sc8])
                                k.op(act, lambda: A.activation(out=sc8[:, 2:3], in_=sc8[:, 1:2], func=AF.Sqrt),
                                     [R_sc8], [R_sc8])
                                k.op(dve, lambda: V.reciprocal(out=sc8[:, 3:4], in_=sc8[:, 2:3]), [R_sc8], [R_sc8])
                                k.op(dve, lambda: V.scalar_tensor_tensor(out=so[:, 0:512], in0=tC[:],
                                                                         scalar=sc8[:, 3:4], in1=glng[:],
                                                                         op0=ALU.mult, op1=ALU.mult),
                                     [R_tC, R_sc8, R_glng], [R_so])
                                k.dma(sp, gv_d[r0:r0 + 128, :], so[:, 0:512], [R_so], [R_gv], R_so)
                    k.dma(pool, wf[:], wf_r[l].rearrange("p (k c) -> p k c", c=8), [R_in], [R_wf], R_wf)
                    for tt in range(8):
                        ps, R_ps = k.nextps()
                        k.mm(ps[:, 0:8], [(hb[:, kk, tt * 128:(tt + 1) * 128], wf[:, kk, :]) for kk in range(KC)],
                             [R_wf, R_hb], [R_ps])
                        gt = sbi * 8 + tt
                        k.op(dve, lambda: V.tensor_tensor(out=sc8[:], in0=ps[:, 0:8], in1=fgb[:], op=ALU.add),
                             [R_ps, R_fgb], [R_sc8])
                        k.op(act, lambda: A.activation(out=sc8[:], in_=sc8[:], func=AF.Exp, scale=-1.0),
                             [R_sc8], [R_sc8])
                        k.op(act, lambda: A.activation(out=sc8[:], in_=sc8[:], func=AF.Ln, bias=1.0, scale=1.0),
                             [R_sc8], [R_sc8])
                        k.op(dve, lambda: V.tensor_scalar(out=lf_sb[:, gt, :], in0=sc8[:], scalar1=-1.0, scalar2=None,
                                                          op0=ALU.mult), [R_sc8], [R_lf])
                k.barrier()

            with ExitStack() as st:
                lfv = lf_sb[:].rearrange("p n h -> p (n h)")
                for c0 in range(0, NT * 8, 512):
                    w_ = min(512, NT * 8 - c0)
                    ps, R_ps = k.nextps()
                    k.mm(ps[:, 0:w_], [(tril_f[:], lfv[:, c0:c0 + w_])], [R_trilf, R_lf], [R_ps])
                    k.op(dve, lambda: V.tensor_copy(out=within[:].rearrange("p n h -> p (n h)")[:, c0:c0 + w_],
                                                    in_=ps[:, 0:w_]), [R_ps], [R_wi])
                    ps, R_ps = k.nextps()
                    k.mm(ps[:, 0:w_], [(ones_f[:], lfv[:, c0:c0 + w_])], [R_onesf, R_lf], [R_ps])
                    k.op(dve, lambda: V.tensor_copy(out=tot[:].rearrange("p n h -> p (n h)")[:, c0:c0 + w_],
                                                    in_=ps[:, 0:w_]), [R_ps], [R_tot])
                k.op(dve, lambda: V.tensor_copy(out=pinc[:, :, 0], in_=tot[:, 0, :]), [R_tot], [R_pinc])
                k.op(dve, lambda: V.tensor_scalar(out=negF[:, 0, :], in0=within[:, 0, :], scalar1=-1.0, scalar2=None,
                                                  op0=ALU.mult), [R_wi], [R_negF])
                for j in range(1, NT):
                    k.op(dve, lambda: V.tensor_tensor(out=pinc[:, :, j], in0=pinc[:, :, j - 1], in1=tot[:, j, :],
                                                      op=ALU.add), [R_pinc, R_tot], [R_pinc])
                    k.op(dve, lambda: V.scalar_tensor_tensor(out=negF[:, j, :], in0=within[:, j, :], scalar=-1.0,
                                                             in1=pinc[:, :, j - 1], op0=ALU.mult, op1=ALU.subtract),
                         [R_wi, R_pinc], [R_negF])
                if split:
                    xs, R_xs = k.sb("x_xs", [128, 320], F32, st)
                    hz, R_hz = k.sb("x_hz", [128, 4, 15], F32, st)
                    hg, R_hg = k.sb("x_hg", [128, 4, 30], BF16, st)
                    k.op(dve, lambda: V.memset(xs[:], 0.0), [], [R_xs])
                    k.op(dve, lambda: V.tensor_copy(out=xs[:, 0:NT * 8], in_=negF[:].rearrange("p n h -> p (n h)")),
                         [R_negF], [R_xs])
                    k.op(dve, lambda: V.tensor_copy(out=xs[:, 128:136], in_=pinc[:, :, NT - 1]), [R_pinc], [R_xs])
                    k.dma(sp, hz[:], zp_d.rearrange("(g p) t -> p g t", p=128)[:, :, T - 15:T], [R_zp], [R_hz], R_hz)
                    k.dma(sp, hg[:], glu_d.rearrange("(g p) t -> p g t", p=128)[:, :, T - 30:T], [R_glu], [R_hg], R_hg)
                    k.op(dve, lambda: V.tensor_copy(out=xs[:, 136:196].rearrange("p (g t) -> p g t", g=4), in_=hz[:]),
                         [R_hz], [R_xs])
                    k.op(dve, lambda: V.tensor_copy(out=xs[:, 196:316].rearrange("p (g t) -> p g t", g=4), in_=hg[:]),
                         [R_hg], [R_xs])
                    k.dma(sp, xs_d[l % 2][:, :], xs[:], [R_xs], [R_xsd], R_xs)
                    coll(xs_d[l % 2][:, :], gs_d[l % 2][:, :], [R_xsd], [R_gs])
                    for q in range(4):
                        coll(xkv[l % 2][q][:, :], gkv[l % 2][q][:, :], [R_k, R_v], [R_gkv])
                k.barrier()

            with ExitStack() as st:
                zb = [k.sb(f"p_z{i}", [128, 4, 527], F32, st) for i in range(2)]
                pa, R_pa = k.sb("p_a", [128, 527], F32, st)
                pb, R_pb = k.sb("p_b", [128, 527], F32, st)
                mx = [k.sb(f"p_mx{i}", [128, 512], BF16, st) for i in range(2)]
                pw, R_pw = k.sb("p_w", [128, 4, 128], BF16, st)
                psc, R_psc = k.sb("p_sc", [128, 4], F32, st)
                icn, R_icn = k.sb("p_icn", [128, 4, 512], F32, st)
                so = [k.sb(f"p_so{i}", [128, 4, 512], BF16, st) for i in range(2)]
                k.dma(pool, pw[:], pool_w[l].rearrange("g c d -> c g d"), [R_in], [R_pw], R_pw)
                k.dma(sp, psc[:], pscale_col[l], [R_in], [R_psc], R_psc)
                k.dma(sp, icn[:], c_invcnt, [R_in], [R_icn], R_icn)
                zpv = zp_d.rearrange("(g p) t -> p g t", p=128)
                yv = yT_d.rearrange("(g p) t -> p g t", p=128)
                for b in range(NB):
                    z, R_z = zb[b % 2]
                    sot, R_so = so[b % 2]
                    if b == 0:
                        if split:
                            hz2, R_hz2 = k.sb("p_hz2", [128, 60], F32, st)
                            k.dma(sp, hz2[:], gs_d[l % 2][0:128, 136:196], [R_gs], [R_hz2], R_hz2)
                            k.op(dve, lambda: V.tensor_scalar(out=z[:, :, 0:15],
                                                              in0=hz2[:].rearrange("p (g t) -> p g t", g=4),
                                                              scalar1=flagc[:, 0:1], scalar2=None, op0=ALU.mult),
                                 [R_hz2, R_flag], [R_z])
                        else:
                            k.op(dve, lambda: V.memset(z[:, :, 0:15], 0.0), [], [R_z])
                        k.dma(sp, z[:, :, 15:527], zpv[:, :, 0:512], [R_zp], [R_z], R_z)
                    else:
                        k.dma(sp, z[:, :, :], zpv[:, :, b * 512 - 15:b * 512 + 512], [R_zp], [R_z], R_z)
                    for g in range(4):
                        src = z[:, g, :]
                        bufs = [(pa, R_pa), (pb, R_pb)]
                        cur, R_cur = None, R_z
                        for s in range(g + 1):
                            sh = 1 << s
                            lo = 2 * sh - 1
                            dstb, R_d = bufs[s % 2]
                            srcap = src if cur is None else cur
                            k.op(dve, lambda: V.tensor_tensor(out=dstb[:, lo:527], in0=srcap[:, lo:527],
                                                              in1=srcap[:, lo - sh:527 - sh], op=ALU.add),
                                 [R_cur], [R_d])
                            cur, R_cur = dstb, R_d
                        mt, R_mt = mx[g % 2]
                        w = 2 << g
                        if b == 0:
                            k.op(dve, lambda: V.tensor_tensor(out=cur[:, 15:527], in0=cur[:, 15:527], in1=icn[:, g, :],
                                                              op=ALU.mult), [R_cur, R_icn], [R_cur])
                            k.op(dve, lambda: V.tensor_tensor(out=mt[:], in0=cur[:, 15:527], in1=z[:, g, 15:527],
                                                              op=ALU.subtract), [R_cur, R_z], [R_mt])
                        else:
                            k.op(dve, lambda: V.scalar_tensor_tensor(out=mt[:], in0=cur[:, 15:527], scalar=1.0 / w,
                                                                     in1=z[:, g, 15:527], op0=ALU.mult,
                                                                     op1=ALU.subtract), [R_cur, R_z], [R_mt])
                        ps, R_ps = k.nextps()
                        k.mm(ps[:], [(pw[:, g, :], mt[:])], [R_pw, R_mt], [R_ps])
                        k.op(act, lambda: A.activation(out=sot[:, g, :], in_=ps[:], func=AF.Identity, bias=0.0,
                                                       scale=psc[:, g:g + 1]), [R_ps, R_psc], [R_so])
                    k.dma(sp, yv[:, 0:4, b * 512:(b + 1) * 512], sot[:], [R_so], [R_y], R_so)
                k.barrier()

            with ExitStack() as st:
                gb = [k.sb(f"v_g{i}", [128, 4, 542], BF16, st) for i in range(2)]
                dg, R_dg = k.sb("v_dg", [128, 124, 128], BF16, st)
                cw, R_cw = k.sb("v_cw", [128, 4, 31], F32, st)
                cb, R_cb = k.sb("v_cb", [128, 4], F32, st)
                lg, R_lg = k.sb("v_lg", [128, 4], F32, st)
                lb, R_lb = k.sb("v_lb", [128, 4], F32, st)
                yc, R_yc = k.sb("v_yc", [128, 4, 512], F32, st)
                ysq = [k.sb(f"v_ysq{i}", [128, 512], F32, st) for i in range(2)]
                mean, R_mean = k.sb("v_mean", [128, 512], F32, st)
                rstd, R_rstd = k.sb("v_rstd", [128, 512], F32, st)
                tq, R_tq = k.sb("v_tq", [128, 512], F32, st)
                so = [k.sb(f"v_so{i}", [128, 4, 512], BF16, st) for i in range(2)]
                k.dma(sp, cw[:], convw_col[l], [R_in], [R_cw], R_cw)
                k.dma(sp, cb[:], convb_col[l], [R_in], [R_cb], R_cb)
                k.dma(sp, lg[:], clng_col[l], [R_in], [R_lg], R_lg)
                k.dma(sp, lb[:], clnb_col[l], [R_in], [R_lb], R_lb)
                for c in range(4):
                    for j in range(31):
                        k.op(dve, lambda: V.tensor_scalar(out=dg[:, c * 31 + j, :], in0=identb[:],
                                                          scalar1=cw[:, c, j:j + 1], scalar2=None, op0=ALU.mult),
                             [R_identb, R_cw], [R_dg])
                gv_ = glu_d.rearrange("(g p) t -> p g t", p=128)
                yv = yT_d.rearrange("(g p) t -> p g t", p=128)
                for b in range(NB):
                    g_, R_g = gb[b % 2]
                    sot, R_so = so[b % 2]
                    if b == 0:
                        if split:
                            hg2, R_hg2 = k.sb("v_hg2", [128, 120], F32, st)
                            k.dma(sp, hg2[:], gs_d[l % 2][0:128, 196:316], [R_gs], [R_hg2], R_hg2)
                            k.op(dve, lambda: V.tensor_scalar(out=g_[:, :, 0:30],
                                                              in0=hg2[:].rearrange("p (g t) -> p g t", g=4),
                                                              scalar1=flagc[:, 0:1], scalar2=None, op0=ALU.mult),
                                 [R_hg2, R_flag], [R_g])
                        else:
                            k.op(dve, lambda: V.memset(g_[:, :, 0:30], 0.0), [], [R_g])
                        k.dma(sp, g_[:, :, 30:542], gv_[:, :, 0:512], [R_glu], [R_g], R_g)
                    else:
                        k.dma(sp, g_[:, :, :], gv_[:, :, b * 512 - 30:b * 512 + 512], [R_glu], [R_g], R_g)
                    psA, R_psA = k.nextps()
                    psB, R_psB = k.nextps()
                    for c in range(4):
                        ps, R_ps = k.nextps()
                        k.mm(ps[:], [(dg[:, c * 31 + j, :], g_[:, c, j:j + 512]) for j in range(31)],
                             [R_dg, R_g], [R_ps])
                        k.op(act, lambda: A.activation(out=yc[:, c, :], in_=ps[:], func=AF.Identity,
                                                       bias=cb[:, c:c + 1], scale=1.0), [R_ps, R_cb], [R_yc])
                        yq, R_yq = ysq[c % 2]
                        k.op(act, lambda: A.activation(out=yq[:], in_=yc[:, c, :], func=AF.Square), [R_yc], [R_yq])
                        k.mm1(psA[:], ones_f[:], yc[:, c, :], c == 0, c == 3, [R_onesf, R_yc], [R_psA])
                        k.mm1(psB[:], ones_f[:], yq[:], c == 0, c == 3, [R_onesf, R_yq], [R_psB])
                    k.op(dve, lambda: V.tensor_scalar(out=mean[:], in0=psA[:], scalar1=1.0 / 512, scalar2=None,
                                                      op0=ALU.mult), [R_psA], [R_mean])
                    k.op(dve, lambda: V.tensor_tensor(out=tq[:], in0=mean[:], in1=mean[:], op=ALU.mult),
                         [R_mean], [R_tq])
                    k.op(dve, lambda: V.scalar_tensor_tensor(out=rstd[:], in0=psB[:], scalar=1.0 / 512, in1=tq[:],
                                                             op0=ALU.mult, op1=ALU.subtract), [R_psB, R_tq], [R_rstd])
                    k.op(dve, lambda: V.tensor_scalar(out=rstd[:], in0=rstd[:], scalar1=EPS, scalar2=None,
                                                      op0=ALU.add), [R_rstd], [R_rstd])
                    k.op(act, lambda: A.activation(out=rstd[:], in_=rstd[:], func=AF.Sqrt), [R_rstd], [R_rstd])
                    k.op(dve, lambda: V.reciprocal(out=rstd[:], in_=rstd[:]), [R_rstd], [R_rstd])
                    for c in range(4):
                        k.op(dve, lambda: V.tensor_tensor(out=yc[:, c, :], in0=yc[:, c, :], in1=mean[:],
                                                          op=ALU.subtract), [R_yc, R_mean], [R_yc])
                        k.op(dve, lambda: V.tensor_tensor(out=yc[:, c, :], in0=yc[:, c, :], in1=rstd[:],
                                                          op=ALU.mult), [R_yc, R_rstd], [R_yc])
                        k.op(act, lambda: A.activation(out=sot[:, c, :], in_=yc[:, c, :], func=AF.Silu,
                                                       bias=lb[:, c:c + 1], scale=lg[:, c:c + 1]),
                             [R_yc, R_lb, R_lg], [R_so])
                    k.dma(sp, yv[:, 4:8, b * 512:(b + 1) * 512], sot[:], [R_so], [R_y], R_so)
                k.barrier()

            with ExitStack() as st:
                npair = NT * (NT + 1) // 2
                bias_t, R_bias = k.sb("a_bias", [128, npair], F32, st)
                kt = [k.sb(f"a_k{i}", [128, T], BF16, st) for i in range(2)]
                qt = [k.sb(f"a_q{i}", [128, T], BF16, st) for i in range(2)]
                vt = [k.sb(f"a_v{i}", [128, NT, 128], BF16, st) for i in range(2)]
                pT = []
                for i in range(3):
                    t_, _r = k.sb(f"a_p{i}", [128, 512], BF16, st)
                    pT.append((t_, [Res(f"a_p{i}_{jj}") for jj in range(4)]))
                rc, R_rc = k.sb("a_rc", [128, 512], F32, st)
                so = [k.sb(f"a_so{i}", [128, 512], BF16, st) for i in range(2)]
                if split:
                    ktp = [k.sb(f"a_kp{i}", [128, T], BF16, st) for i in range(2)]
                    vtp = [k.sb(f"a_vp{i}", [128, NT, 128], BF16, st) for i in range(2)]
                    bias_p, R_biasp = k.sb("a_biasp", [128, NT * NT], F32, st)
                    gsm, R_gsm = k.sb("a_gsm", [128, 136], F32, st)
                    tp, R_tp = k.sb("a_tp", [128, 8], F32, st)
                    cv, R_cv = k.sb("a_cv", [128, NT, 8], F32, st)
                    k.dma(sp, gsm[:], gs_d[l % 2][0:128, 0:136], [R_gs], [R_gsm], R_gsm)
                    k.op(dve, lambda: V.tensor_scalar(out=tp[:], in0=gsm[:, 128:136], scalar1=flagc[:, 0:1],
                                                      scalar2=pmbc[:, 0:1], op0=ALU.mult, op1=ALU.add),
                         [R_gsm, R_flag, R_pmb], [R_tp])
                    for i in range(NT):
                        k.op(dve, lambda: V.tensor_tensor(out=cv[:, i, :], in0=gsm[:, i * 8:(i + 1) * 8], in1=tp[:],
                                                          op=ALU.add), [R_gsm, R_tp], [R_cv])
                    gka = gkv[l % 2][0][0:CH, :].rearrange("(a b) c -> a (b c)", b=T // 1024)
                    gkb = gkv[l % 2][1][0:CH, :].rearrange("(a b) c -> a (b c)", b=T // 1024)
                    gva = gkv[l % 2][2][0:CH, :].rearrange("(n p) c -> p n c", p=128)
                    gvb_ = gkv[l % 2][3][0:CH, :].rearrange("(n p) c -> p n c", p=128)
                yv = yT_d.rearrange("(g p) t -> p g t", p=128)
                vva = v_a.rearrange("(n p) c -> p n c", p=128)
                vvb = v_b.rearrange("(n p) c -> p n c", p=128)
                NH = NT // 2
                pcnt = 0
                for h in range(8):
                    ktt, R_kt = kt[h % 2]
                    qtt, R_qt = qt[h % 2]
                    vtt, R_vt = vt[h % 2]
                    ksrc_d = kTa if h < 4 else kTb
                    hr = (h % 4) * 128
                    k.dma(sp, ktt[:], ksrc_d[hr:hr + 128, :], [R_k], [R_kt], R_kt)
                    k.dma(sp, qtt[:], qT_d[h * 128:(h + 1) * 128, :], [R_q], [R_qt], R_qt)
                    k.dma(sp, vtt[:, 0:NH, :], vva[:, :, h * 128:(h + 1) * 128], [R_v], [R_vt], R_vt)
                    k.dma(sp, vtt[:, NH:NT, :], vvb[:, :, h * 128:(h + 1) * 128], [R_v], [R_vt], R_vt)
                    if split:
                        ktpt, R_ktp = ktp[h % 2]
                        vtpt, R_vtp = vtp[h % 2]
                        gk = gka if h < 4 else gkb
                        k.dma(sp, ktpt[:], gk[hr:hr + 128, :], [R_gkv], [R_ktp], R_ktp)
                        k.dma(sp, vtpt[:, 0:NH, :], gva[:, :, h * 128:(h + 1) * 128], [R_gkv], [R_vtp], R_vtp)
                        k.dma(sp, vtpt[:, NH:NT, :], gvb_[:, :, h * 128:(h + 1) * 128], [R_gkv], [R_vtp], R_vtp)
                        for i in range(NT):
                            k.op(dve, lambda: V.tensor_scalar(out=bias_p[:, i * NT:(i + 1) * NT], in0=pinc[:, h, 0:NT],
                                                              scalar1=cv[:, i, h:h + 1], scalar2=None, op0=ALU.add),
                                 [R_pinc, R_cv], [R_biasp])
                    offs = []
                    o = 0
                    for i in range(NT):
                        offs.append(o)
                        k.op(dve, lambda: V.tensor_scalar(out=bias_t[:, o:o + NT - i], in0=pinc[:, h, i:NT],
                                                          scalar1=negF[:, i, h:h + 1], scalar2=None, op0=ALU.add),
                             [R_pinc, R_negF], [R_bias])
                        o += NT - i
                    steps_all = []
                    for qb in range(NB):
                        steps = ([("p", i) for i in range(NT)] if split else []) + [("o", i) for i in range(4 * qb + 4)]
                        for si, (kind, i) in enumerate(steps):
                            steps_all.append((qb, kind, i, si == 0, si == len(steps) - 1))

                    def step_info(n):
                        qb, kind, i, first, last = steps_all[n]
                        kk_ = i - 4 * qb if kind == "o" else -1
                        j0 = max(kk_, 0)
                        bidx = (pcnt + n) % 3
                        return qb, kind, i, first, last, j0, bidx

                    def emit_qk(n):
                        qb, kind, i, first, last, j0, bidx = step_info(n)
                        cs = slice(j0 * 128, 512)
                        psS, R_psS = k.ps[bidx], k.psr[bidx]
                        ksrc, R_ks = (ktpt, R_ktp) if kind == "p" else (ktt, R_kt)
                        k.mm(psS[:, cs], [(ksrc[:, i * 128:(i + 1) * 128],
                                           qtt[:, qb * 512 + j0 * 128:(qb + 1) * 512])], [R_ks, R_qt], [R_psS])

                    emit_qk(0)
                    for n in range(len(steps_all)):
                        if n + 1 < len(steps_all):
                            emit_qk(n + 1)
                        qb, kind, i, first, last, j0, bidx = step_info(n)
                        cs = slice(j0 * 128, 512)
                        psS, R_psS = k.ps[bidx], k.psr[bidx]
                        pt, R_pts = pT[bidx]
                        psO, R_psO = k.ps[3 + qb % 2], k.psr[3 + qb % 2]
                        psD, R_psD = k.ps[5 + qb % 2], k.psr[5 + qb % 2]
                        vsrc, R_vs = (vtpt, R_vtp) if kind == "p" else (vtt, R_vt)
                        for jj in range(j0, 4):
                            j = 4 * qb + jj
                            js = slice(jj * 128, (jj + 1) * 128)
                            if kind == "p":
                                bap, R_b = bias_p[:, i * NT + j:i * NT + j + 1], R_biasp
                            else:
                                bi = offs[i] + (j - i)
                                bap, R_b = bias_t[:, bi:bi + 1], R_bias
                            k.op(act, lambda: A.activation(out=pt[:, js], in_=psS[:, js], func=AF.Exp,
                                                           bias=bap, scale=0.08838834764831845),
                                 [R_psS, R_b], [R_pts[jj]])
                            if kind == "o" and j == i:
                                k.op(pool, lambda: G.tensor_tensor(out=pt[:, js], in0=pt[:, js], in1=tril_b[:],
                                                                   op=ALU.mult), [R_pts[jj], R_trilb], [R_pts[jj]])
                        k.mm1(psO[:, cs], vsrc[:, i, :], pt[:, cs], first, last, [R_vs] + R_pts[j0:4], [R_psO])
                        k.mm1(psD[:, cs], ones_b[:], pt[:, cs], first, last, [R_onesb] + R_pts[j0:4], [R_psD])
                        if last:
                            sot, R_so = so[qb % 2]
                            k.op(dve, lambda: V.reciprocal(out=rc[:], in_=psD[:]), [R_psD], [R_rc])
                            k.op(dve, lambda: V.tensor_tensor(out=sot[:], in0=psO[:], in1=rc[:], op=ALU.mult),
                                 [R_psO, R_rc], [R_so])
                            k.dma(sp, yv[:, 8 + h, qb * 512:(qb + 1) * 512], sot[:], [R_so], [R_y], R_so)
                    pcnt += len(steps_all)
                k.psi = 0
                k.barrier()

            with ExitStack() as st:
                wsf, R_wsf = k.sb("g_wsf", [128, 4, 128], F32, st)
                wmT, R_wmT = k.sb("g_wmT", [128, 4, 128], BF16, st)
                bsr, R_bsr = k.sb("g_bsr", [128, 512], F32, st)
                gvb = [k.sb(f"g_gv{i}", [128, 4, 512], BF16, st) for i in range(2)]
                ub = [k.sb(f"g_u{i}", [128, 4, 512], BF16, st) for i in range(2)]
                tg, R_tg = k.sb("g_t", [128, 512], F32, st)
                so = [k.sb(f"g_so{i}", [128, 4, 512], BF16, st) for i in range(2)]
                k.dma(sp, wsf[:], gmlp_ws[l].rearrange("g t s -> t g s"), [R_in], [R_wsf], R_wsf)
                k.dma(sp, bsr[:], gbs_row[l], [R_in], [R_bsr], R_bsr)
                for g in range(4):
                    k.op(dve, lambda: V.tensor_tensor(out=wsf[:, g, :], in0=wsf[:, g, :], in1=trilT_f[:], op=ALU.mult),
                         [R_wsf, R_trilT], [R_wsf])
                ps, R_ps = k.nextps()
                for g in range(4):
                    k.tr(ps[:, g * 128:(g + 1) * 128], wsf[:, g, :], ident[:], [R_wsf, R_ident], [R_ps])
                k.op(dve, lambda: V.tensor_copy(out=wmT[:].rearrange("p g t -> p (g t)"), in_=ps[:]), [R_ps], [R_wmT])
                gvv = gv_d.rearrange("(n p) c -> p n c", p=128)
                uv = uT_d.rearrange("(g p) t -> p g t", p=128)
                yv = yT_d.rearrange("(g p) t -> p g t", p=128)
                for b in range(NB):
                    gvt, R_gvt = gvb[b % 2]
                    ut, R_ut = ub[b % 2]
                    sot, R_so = so[b % 2]
                    k.dma(sp, gvt[:], gvv[:, b * 4:(b + 1) * 4, :], [R_gv], [R_gvt], R_gvt)
                    k.dma(sp, ut[:], uv[:, :, b * 512:(b + 1) * 512], [R_u], [R_ut], R_ut)
                    for n in range(4):
                        ps, R_ps = k.nextps()
                        for g in range(4):
                            k.mm(ps[:, g * 128:(g + 1) * 128], [(gvt[:, n, g * 128:(g + 1) * 128], wmT[:, g, :])],
                                 [R_gvt, R_wmT], [R_ps])
                        k.op(dve, lambda: V.tensor_tensor(out=tg[:], in0=ps[:], in1=bsr[:], op=ALU.add),
                             [R_ps, R_bsr], [R_tg])
                        k.op(dve, lambda: V.tensor_tensor(out=sot[:, :, n * 128:(n + 1) * 128],
                                                          in0=tg[:].rearrange("p (g t) -> p g t", g=4),
                                                          in1=ut[:, :, n * 128:(n + 1) * 128], op=ALU.mult),
                             [R_tg, R_ut], [R_so])
                    k.dma(sp, yv[:, 16:20, b * 512:(b + 1) * 512], sot[:], [R_so], [R_y], R_so)
                k.barrier()

            with ExitStack() as st:
                hb, R_hb = k.sb("e_h", [128, KC, 1024], BF16, st)
                yb, R_yb = k.sb("e_y", [128, 20, 1024], BF16, st)
                mg, R_mg = k.sb("e_mg", [128, KC, 1024], BF16, st)
                wg = [k.sb(f"e_wg{i}", [128, 4, KC, 128], BF16, st) for i in range(2)]
                wb = [k.sb(f"e_wb{i}", [128, 20, 128], BF16, st) for i in range(2)]
                wo = [k.sb(f"e_wo{i}", [128, KC, 128], BF16, st) for i in range(2)]
                bg, R_bg = k.sb("e_bg", [128, 64], F32, st)
                gs = [k.sb(f"e_gs{i}", [128, 512], F32, st) for i in range(2)]
                acc = [k.sb(f"e_acc{i}", [128, 512], F32, st) for i in range(2)]
                tmp = [k.sb(f"e_tmp{i}", [128, 512], F32, st) for i in range(2)]
                xo = [k.sb(f"e_xo{i}", [128, 1024], F32, st) for i in range(2)]
                k.dma(sp, bg[:], bgate_col[l], [R_in], [R_bg], R_bg)
                yv = yT_d.rearrange("(g p) t -> p g t", p=128)
                brk = [(0, 4), (4, 8), (8, 16), (16, 20)]
                cnt = 0
                for sbi in range(NSB):
                    t0 = sbi * 1024
                    k.dma(sp, hb[:], hTv[:, :, t0:t0 + 1024], [R_hT], [R_hb], R_hb)
                    k.dma(sp, yb[:], yv[:, :, t0:t0 + 1024], [R_y], [R_yb], R_yb)
                    def issue_g(j, m_):
                        wgt, R_wg = wg[m_ % 2]
                        wbt, R_wb = wb[m_ % 2]
                        k.dma(pool, wgt[:], w_gate_r[l, m_].rearrange("p (i k c) -> p i k c", i=4, k=KC),
                              [R_in], [R_wg], R_wg)
                        k.dma(pool, wbt[:], w_branch_r[l, m_].rearrange("p (k c) -> p k c", c=128),
                              [R_in], [R_wb], R_wb)
                        return wgt, R_wg, wbt, R_wb
                    getg = prefetcher(list(range(KC)), issue_g, 1)

                    def issue_o(j, m_):
                        wot, R_wo = wo[m_ % 2]
                        k.dma(pool, wot[:], w_o_r[l, m_].rearrange("p (k c) -> p k c", c=128), [R_in], [R_wo], R_wo)
                        return wot, R_wo
                    geto = prefetcher(list(range(KC)), issue_o, 1)
                    for m in range(KC):
                        wgt, R_wg, wbt, R_wb = getg(m)
                        for half in range(2):
                            hs = slice(half * 512, (half + 1) * 512)
                            at, R_at = acc[half]
                            for i in range(4):
                                psg, R_psg = k.nextps()
                                k.mm(psg[:], [(wgt[:, i, kk, :], hb[:, kk, hs]) for kk in range(KC)],
                                     [R_wg, R_hb], [R_psg])
                                gt_, R_gs = gs[cnt % 2]
                                tt_, R_tp = tmp[cnt % 2]
                                cnt += 1
                                k.op(act, lambda: A.activation(out=gt_[:], in_=psg[:], func=AF.Sigmoid,
                                                               bias=bg[:, i * 16 + m:i * 16 + m + 1], scale=1.0),
                                     [R_psg, R_bg], [R_gs])
                                psb, R_psb = k.nextps()
                                k0, k1 = brk[i]
                                k.mm(psb[:], [(wbt[:, kk, :], yb[:, kk, hs]) for kk in range(k0, k1)],
                                     [R_wb, R_yb], [R_psb])
                                if i == 0:
                                    k.op(dve, lambda: V.tensor_tensor(out=at[:], in0=psb[:], in1=gt_[:], op=ALU.mult),
                                         [R_psb, R_gs], [R_at])
                                else:
                                    k.op(dve, lambda: V.tensor_tensor(out=tt_[:], in0=psb[:], in1=gt_[:], op=ALU.mult),
                                         [R_psb, R_gs], [R_tp])
                                    if i < 3:
                                        k.op(pool, lambda: G.tensor_tensor(out=at[:], in0=at[:], in1=tt_[:], op=ALU.add),
                                             [R_at, R_tp], [R_at])
                                    else:
                                        k.op(pool, lambda: G.tensor_tensor(out=mg[:, m, hs], in0=at[:], in1=tt_[:],
                                                                           op=ALU.add), [R_at, R_tp], [R_mg])
                    for m in range(KC):
                        wot, R_wo = geto(m)
                        xot, R_xo = xo[m % 2]
                        k.dma(sp, xot[:], xT[m * 128:(m + 1) * 128, t0:t0 + 1024], [R_xTs[m]], [R_xo], R_xo)
                        for half in range(2):
                            hs = slice(half * 512, (half + 1) * 512)
                            ps, R_ps = k.nextps()
                            k.mm(ps[:], [(wot[:, kk, :], mg[:, kk, hs]) for kk in range(KC)], [R_wo, R_mg], [R_ps])
                            k.op(dve, lambda: V.scalar_tensor_tensor(out=xot[:, hs], in0=ps[:],
                                                                     scalar=modv[:, l, GT1 + m:GT1 + m + 1],
                                                                     in1=xot[:, hs], op0=ALU.mult, op1=ALU.add),
                                 [R_ps, R_mod, R_xo], [R_xo])
                        k.dma(sp, xT[m * 128:(m + 1) * 128, t0:t0 + 1024], xot[:], [R_xo], [R_xTs[m]], R_xo)
                k.barrier()

            if not do_moe:
                continue
            with ExitStack() as st:
                yacc, R_yacc = k.sb("f_yacc", [128, KC, 1024], F32, st)
                h2, R_h2 = k.sb("f_h2", [128, KC, 1024], BF16, st)
                gbc = [k.sb(f"f_gbc{i}", [128, 1024], F32, st) for i in range(2)]
                GT, R_GT = k.sb("f_GT", [32, 1024], F32, st)
                sel = [k.sb(f"f_sel{i}", [32, 128], F32, st) for i in range(2)]
                bup, R_bup = k.sb("f_bup", [128, NE * 12], F32, st)
                k.dma(sp, bup[:], bup_col[l], [R_in], [R_bup], R_bup)
                ucnt = 0
                dcnt = 0
                ecnt = 0
                for sbi in range(NSB):
                    t0 = sbi * 1024
                    st2 = ExitStack()
                    st2.__enter__()
                    rw, R_rw = k.sb("f_rw", [128, KC, NE], F32, st2)
                    rb, R_rb = k.sb("f_rb", [128, 256], F32, st2)
                    bdn, R_bdn = k.sb("f_bdn", [32, D], F32, st2)
                    L, R_L = k.sb("f_L", [128, 256], F32, st2)
                    Gm, R_Gm = k.sb("f_G", [128, 256], F32, st2)
                    m8, R_m8 = k.sb("f_m8", [128, 16], F32, st2)
                    e32, R_e32 = k.sb("f_e32", [128, 64], F32, st2)
                    k.dma(sp, rw[:], router_w[l].rearrange("(k p) e -> p k e", p=128), [R_in], [R_rw], R_rw)
                    k.dma(sp, rb[:], rb_row[l], [R_in], [R_rb], R_rb)
                    k.dma(sp, bdn[:], b_down[l], [R_in], [R_bdn], R_bdn)
                    norm_mod(st2, l, sbi, a2v, SH2, h2, R_h2, h_f32=yacc, R_hf=R_yacc)
                    ps, R_ps = k.nextps()
                    for tt in range(8):
                        k.mm(ps[:, tt * 32:(tt + 1) * 32],
                             [(yacc[:, kk, tt * 128:(tt + 1) * 128], rw[:, kk, :]) for kk in range(KC)],
                             [R_yacc, R_rw], [R_ps])
                    k.op(dve, lambda: V.tensor_tensor(out=L[:], in0=ps[:, 0:256], in1=rb[:], op=ALU.add),
                         [R_ps, R_rb], [R_L])
                    for tt in range(8):
                        ls = slice(tt * 32, (tt + 1) * 32)
                        k.op(dve, lambda: V.max(out=m8[:, 0:8], in_=L[:, ls]), [R_L], [R_m8])
                        k.op(dve, lambda: V.tensor_scalar(out=m8[:, 8:9], in0=m8[:, 0:1], scalar1=-1.0, scalar2=None,
                                                          op0=ALU.mult), [R_m8], [R_m8])
                        k.op(act, lambda: A.activation(out=e32[:, 0:32], in_=L[:, ls], func=AF.Exp,
                                                       bias=m8[:, 8:9], scale=1.0), [R_L, R_m8], [R_e32])
                        k.op(dve, lambda: V.tensor_scalar(out=e32[:, 32:64], in0=L[:, ls], scalar1=m8[:, 3:4],
                                                          scalar2=None, op0=ALU.is_ge), [R_L, R_m8], [R_e32])
                        k.op(dve, lambda: V.tensor_tensor(out=e32[:, 0:32], in0=e32[:, 0:32], in1=e32[:, 32:64],
                                                          op=ALU.mult), [R_e32], [R_e32])
                        k.op(dve, lambda: V.reduce_sum(out=m8[:, 9:10], in_=e32[:, 0:32], axis=AX.X), [R_e32], [R_m8])
                        k.op(dve, lambda: V.reciprocal(out=m8[:, 10:11], in_=m8[:, 9:10]), [R_m8], [R_m8])
                        k.op(dve, lambda: V.tensor_scalar(out=Gm[:, ls], in0=e32[:, 0:32], scalar1=m8[:, 10:11],
                                                          scalar2=None, op0=ALU.mult), [R_e32, R_m8], [R_Gm])
                    for half in range(2):
                        ps, R_ps = k.nextps()
                        for t4 in range(4):
                            tt = half * 4 + t4
                            k.tr(ps[0:32, t4 * 128:(t4 + 1) * 128], Gm[:, tt * 32:(tt + 1) * 32], ident[:],
                                 [R_Gm, R_ident], [R_ps])
                        k.op(dve, lambda: V.tensor_copy(out=GT[:, half * 512:(half + 1) * 512], in_=ps[0:32, :]),
                             [R_ps], [R_GT])
                    for m in range(KC):
                        for half in range(2):
                            hs = slice(half * 512, (half + 1) * 512)
                            ps, R_ps = k.nextps()
                            k.mm(ps[:], [(bdn[:, m * 128:(m + 1) * 128], GT[:, hs])], [R_bdn, R_GT], [R_ps])
                            k.op(act, lambda: A.copy(out=yacc[:, m, hs], in_=ps[:]), [R_ps], [R_yacc])
                    k.barrier()
                    st2.__exit__(None, None, None)
                    st3 = ExitStack()
                    st3.__enter__()
                    actT, R_actT = k.sb("f_act", [128, 6, 1024], BF16, st3)
                    wu = [k.sb(f"f_wu{i}", [128, KC, 256], BF16, st3) for i in range(3)]
                    wd = [k.sb(f"f_wd{i}", [128, 6, 1024], BF16, st3) for i in range(2)]
                    tg_ = [k.sb(f"f_tg{i}", [128, 512], F32, st3) for i in range(2)]
                    ts_ = [k.sb(f"f_ts{i}", [128, 512], F32, st3) for i in range(2)]
                    tl_ = [k.sb(f"f_tl{i}", [128, 512], F32, st3) for i in range(2)]
                    jobs = []
                    for e in range(NE):
                        jobs += [("u", e, c) for c in range(6)] + [("d", e, mh) for mh in range(2)]
                    cnts = {"u": 0, "d": 0}

                    def issue_w(j, job):
                        kind, e, c = job
                        if kind == "u":
                            wut, R_wu = wu[cnts["u"] % 3]
                            cnts["u"] += 1
                            k.dma(pool, wut[:], w_up_r[l, e, c].rearrange("p (k c) -> p k c", c=256),
                                  [R_in], [R_wu], R_wu)
                            return wut, R_wu
                        wdt, R_wd = wd[cnts["d"] % 2]
                        cnts["d"] += 1
                        k.dma(pool, wdt[:], w_down_r[l, e, c].rearrange("p (k c) -> p k c", c=1024),
                              [R_in], [R_wd], R_wd)
                        return wdt, R_wd
                    getw = prefetcher(jobs, issue_w, 2)
                    for e in range(NE):
                        st_, R_sel = sel[e % 2]
                        k.op(dve, lambda: V.tensor_scalar(out=st_[:], in0=iota32[:], scalar1=float(e), scalar2=None,
                                                          op0=ALU.is_equal), [R_iota], [R_sel])
                        gb_, R_gb = gbc[e % 2]
                        for half in range(2):
                            hs = slice(half * 512, (half + 1) * 512)
                            ps, R_ps = k.nextps()
                            k.mm(ps[:], [(st_[:], GT[:, hs])], [R_sel, R_GT], [R_ps])
                            k.op(act, lambda: A.copy(out=gb_[:, hs], in_=ps[:]), [R_ps], [R_gb])
                        for c in range(6):
                            wut, R_wu = getw(e * 8 + c)
                            for half in range(2):
                                hs = slice(half * 512, (half + 1) * 512)
                                psg, R_psg = k.nextps()
                                k.mm(psg[:], [(wut[:, kk, 0:128], h2[:, kk, hs]) for kk in range(KC)],
                                     [R_wu, R_h2], [R_psg])
                                psl, R_psl = k.nextps()
                                k.mm(psl[:], [(wut[:, kk, 128:256], h2[:, kk, hs]) for kk in range(KC)],
                                     [R_wu, R_h2], [R_psl])
                                tg, R_tg = tg_[ecnt % 2]
                                ts, R_ts = ts_[ecnt % 2]
                                tl, R_tl = tl_[ecnt % 2]
                                ecnt += 1
                                bgc = bup[:, e * 12 + c:e * 12 + c + 1]
                                blc = bup[:, e * 12 + 6 + c:e * 12 + 6 + c + 1]
                                k.op(dve, lambda: V.tensor_scalar(out=tg[:], in0=psg[:], scalar1=bgc, scalar2=7.0,
                                                                  op0=ALU.add, op1=ALU.min), [R_psg, R_bup], [R_tg])
                                k.op(act, lambda: A.activation(out=ts[:], in_=tg[:], func=AF.Sigmoid, scale=1.702),
                                     [R_tg], [R_ts])
                                k.op(dve, lambda: V.tensor_scalar(out=tl[:], in0=psl[:], scalar1=blc, scalar2=7.0,
                                                                  op0=ALU.add, op1=ALU.min), [R_psl, R_bup], [R_tl])
                                k.op(dve, lambda: V.tensor_scalar(out=tl[:], in0=tl[:], scalar1=-7.0, scalar2=1.0,
                                                                  op0=ALU.max, op1=ALU.add), [R_tl], [R_tl])
                                k.op(pool, lambda: G.tensor_tensor(out=tg[:], in0=tg[:], in1=ts[:], op=ALU.mult),
                                     [R_tg, R_ts], [R_tg])
                                k.op(pool, lambda: G.tensor_tensor(out=tl[:], in0=tl[:], in1=gb_[:, hs], op=ALU.mult),
                                     [R_tl, R_gb], [R_tl])
                                k.op(dve, lambda: V.tensor_tensor(out=actT[:, c, hs], in0=tg[:], in1=tl[:],
                                                                  op=ALU.mult), [R_tg, R_tl], [R_actT])
                        for mh in range(2):
                            wdt, R_wd = getw(e * 8 + 6 + mh)
                            for m8_ in range(8):
                                m = mh * 8 + m8_
                                for half in range(2):
                                    hs = slice(half * 512, (half + 1) * 512)
                                    ps, R_ps = k.nextps()
                                    k.mm(ps[:], [(wdt[:, c, m8_ * 128:(m8_ + 1) * 128], actT[:, c, hs])
                                                 for c in range(6)], [R_wd, R_actT], [R_ps])
                                    k.op(dve, lambda: V.tensor_tensor(out=yacc[:, m, hs], in0=yacc[:, m, hs],
                                                                      in1=ps[:], op=ALU.add), [R_ps, R_yacc], [R_yacc])
                    for m in range(KC):
                        xot, R_xo = gbc[m % 2]
                        k.dma(sp, xot[:], xT[m * 128:(m + 1) * 128, t0:t0 + 1024], [R_xTs[m]], [R_xo], R_xo)
                        k.op(dve, lambda: V.scalar_tensor_tensor(out=xot[:], in0=yacc[:, m, :],
                                                                 scalar=modv[:, l, GT2 + m:GT2 + m + 1], in1=xot[:],
                                                                 op0=ALU.mult, op1=ALU.add),
                             [R_yacc, R_mod, R_xo], [R_xo])
                        k.dma(sp, xT[m * 128:(m + 1) * 128, t0:t0 + 1024], xot[:], [R_xo], [R_xTs[m]], R_xo)
                    k.barrier()
                    st3.__exit__(None, None, None)

        with ExitStack() as st:
            gf, R_gf = k.sb("z_gf", [128, KC], F32, st)
            xb, R_xb = k.sb("z_x", [128, KC, 512], F32, st)
            sq = [k.sb(f"z_sq{i}", [128, 512], F32, st) for i in range(2)]
            rs, R_rs = k.sb("z_rs", [128, 512], F32, st)
            ob = [k.sb(f"z_o{i}", [128, 4, D], F32, st) for i in range(2)]
            k.dma(sp, gf[:], gfin_col, [R_in], [R_gf], R_gf)
            ov = out_d.rearrange("(n p) d -> p n d", p=128)
            for b in range(NB):
                k.dma(sp, xb[:], xTv[:, :, b * 512:(b + 1) * 512], R_xTs, [R_xb], R_xb)
                obt, R_ob = ob[b % 2]
                if final:
                    ps, R_ps = k.nextps()
                    for kk in range(KC):
                        sqt, R_sq = sq[kk % 2]
                        k.op(act, lambda: A.activation(out=sqt[:], in_=xb[:, kk, :], func=AF.Square), [R_xb], [R_sq])
                        k.mm1(ps[:], ones_f[:], sqt[:], kk == 0, kk == KC - 1, [R_onesf, R_sq], [R_ps])
                    k.op(dve, lambda: V.tensor_scalar(out=rs[:], in0=ps[:], scalar1=1.0 / D, scalar2=EPS,
                                                      op0=ALU.mult, op1=ALU.add), [R_ps], [R_rs])
                    k.op(act, lambda: A.activation(out=rs[:], in_=rs[:], func=AF.Sqrt), [R_rs], [R_rs])
                    k.op(dve, lambda: V.reciprocal(out=rs[:], in_=rs[:]), [R_rs], [R_rs])
                    for kk in range(KC):
                        k.op(dve, lambda: V.scalar_tensor_tensor(out=xb[:, kk, :], in0=xb[:, kk, :],
                                                                 scalar=gf[:, kk:kk + 1], in1=rs[:],
                                                                 op0=ALU.mult, op1=ALU.mult),
                             [R_xb, R_gf, R_rs], [R_xb])
                for tt in range(4):
                    for k4 in range(4):
                        ps, R_ps = k.nextps()
                        for q4 in range(4):
                            kk = k4 * 4 + q4
                            k.tr(ps[:, q4 * 128:(q4 + 1) * 128], xb[:, kk, tt * 128:(tt + 1) * 128], ident[:],
                                 [R_xb, R_ident], [R_ps])
                        if k4 % 2 == 0:
                            k.op(dve, lambda: V.tensor_copy(out=obt[:, tt, k4 * 512:(k4 + 1) * 512], in_=ps[:]),
                                 [R_ps], [R_ob])
                        else:
                            k.op(act, lambda: A.copy(out=obt[:, tt, k4 * 512:(k4 + 1) * 512], in_=ps[:]),
                                 [R_ps], [R_ob])
                k.dma(sp, ov[:, b * 4:(b + 1) * 4, :], obt[:], [R_ob], [R_out], R_ob)
            k.barrier()
    return nc


def _col(v):
    return np.ascontiguousarray(v.reshape(-1, 128).T)


def prep_shared(inp):
    f = np.float32
    L = DEPTH
    sh = {}
    sh["w_ada"] = inp["w_ada"]
    sh["b_ada_col"] = np.stack([_col(inp["b_ada"][l]) for l in range(L)])
    sh["gmix_col"] = np.stack([_col(inp["g_norm_mix"][l]) for l in range(L)])
    wi = inp["w_in"]
    wic = np.concatenate([wi[:, :, 0:O_F], wi[:, :, O_GU:IN_DIM]], axis=2)
    sh["w_in_r"] = wic.reshape(L, KC, 128, 11, 512).transpose(0, 3, 2, 1, 4).reshape(L, 11, 128, KC * 512)
    sh["wf_r"] = wi[:, :, O_F:O_F + 8].reshape(L, KC, 128, 8).transpose(0, 2, 1, 3).reshape(L, 128, KC * 8)
    sh["pool_w"] = inp["pool_w"]
    sh["pscale_col"] = np.stack([_col(inp["pool_scale"][l]) for l in range(L)])
    sh["convw_col"] = np.ascontiguousarray(
        inp["conv_w"].reshape(L, 31, 4, 128).transpose(0, 3, 2, 1))
    sh["convb_col"] = np.stack([_col(inp["conv_b"][l]) for l in range(L)])
    sh["clng_col"] = np.stack([_col(inp["conv_ln_g"][l]) for l in range(L)])
    sh["clnb_col"] = np.stack([_col(inp["conv_ln_b"][l]) for l in range(L)])
    sh["fgb_row"] = np.ascontiguousarray(np.broadcast_to(inp["fgate_b"][:, None, :], (L, 128, 8)))
    sh["glng_row"] = np.ascontiguousarray(np.broadcast_to(inp["gmlp_ln_g"][:, None, :], (L, 128, 512)))
    sh["gmlp_ws"] = inp["gmlp_ws"]
    sh["gbs_row"] = np.ascontiguousarray(
        np.broadcast_to(inp["gmlp_bs"].reshape(L, 1, 512), (L, 128, 512)))
    sh["w_gate_r"] = inp["w_gate"].reshape(L, KC, 128, 4, KC, 128).transpose(0, 4, 2, 3, 1, 5).reshape(
        L, KC, 128, 4 * KC * 128)
    sh["bgate_col"] = np.stack([_col(inp["b_gate"][l]) for l in range(L)])
    sh["w_branch_r"] = inp["w_branch"].reshape(L, 20, 128, KC, 128).transpose(0, 3, 2, 1, 4).reshape(
        L, KC, 128, 20 * 128)
    sh["w_o_r"] = inp["w_o"].reshape(L, KC, 128, KC, 128).transpose(0, 3, 2, 1, 4).reshape(L, KC, 128, KC * 128)
    sh["gffn_col"] = np.stack([_col(inp["g_norm_ffn"][l]) for l in range(L)])
    sh["router_w"] = inp["router_w"]
    sh["rb_row"] = np.ascontiguousarray(
        np.broadcast_to(np.tile(inp["router_b"], (1, 8))[:, None, :], (L, 128, 256)))
    sh["w_up_r"] = inp["w_up"].reshape(L, NE, KC, 128, 2, 6, 128).transpose(0, 1, 5, 3, 2, 4, 6).reshape(
        L, NE, 6, 128, KC * 256)
    sh["bup_col"] = np.ascontiguousarray(
        inp["b_up"].reshape(L, NE, 12, 128).transpose(0, 3, 1, 2).reshape(L, 128, NE * 12))
    sh["w_down_r"] = inp["w_down"].reshape(L, NE, 6, 128, 2, 1024).transpose(0, 1, 4, 3, 2, 5).reshape(
        L, NE, 2, 128, 6 * 1024)
    sh["b_down"] = inp["b_down"]
    sh["gfin_col"] = _col(inp["g_final"])
    sh["c_ident"] = np.eye(128, dtype=f)
    sh["c_tril"] = np.triu(np.ones((128, 128), f))
    sh["c_trilT"] = np.tril(np.ones((128, 128), f))
    t = np.arange(512)
    ic = np.stack([1.0 / np.minimum(t + 1, w) for w in (2, 4, 8, 16)]).astype(f)
    sh["c_invcnt"] = np.ascontiguousarray(np.broadcast_to(ic[None], (128, 4, 512)))
    sh["c_iota32"] = np.ascontiguousarray(np.broadcast_to(np.arange(32, dtype=f)[:, None], (32, 128)))
    return {k_: np.ascontiguousarray(v, dtype=f) for k_, v in sh.items()}


SPLIT = True


def kernel(**inputs):
    inp = {k_: np.asarray(v) for k_, v in inputs.items()}
    x = inp["x"].astype(np.float32)
    c = inp["c"].astype(np.float32)
    B = x.shape[0]
    sh = prep_shared(inp)
    in_maps = []
    if SPLIT:
        T = SEQ // 2
        nc = build(T, split=True)
        ic2 = np.ascontiguousarray(np.broadcast_to(
            np.array([1.0 / w for w in (2, 4, 8, 16)], np.float32)[None, :, None], (128, 4, 512)))
        for core in range(8):
            b, half = core // 2, core % 2
            m = dict(sh)
            m["x"] = np.ascontiguousarray(x[b, half * T:(half + 1) * T])
            m["c_col"] = _col(c[b])
            m["flag_col"] = np.full((128, 1), float(half), np.float32)
            m["pmb_col"] = np.full((128, 1), 0.0 if half else -30000.0, np.float32)
            if half:
                m["c_invcnt"] = ic2
            in_maps.append(m)
        res = run_bass_kernel_spmd(nc, in_maps, core_ids=list(range(8)))
        out = np.stack([np.concatenate([res.results[2 * b]["out"], res.results[2 * b + 1]["out"]], axis=0)
                        for b in range(B)], axis=0)
        return out.astype(np.float32)
    nc = build(SEQ)
    for core in range(8):
        b = core % B
        m = dict(sh)
        m["x"] = np.ascontiguousarray(x[b])
        m["c_col"] = _col(c[b])
        m["flag_col"] = np.zeros((128, 1), np.float32)
        m["pmb_col"] = np.zeros((128, 1), np.float32)
        in_maps.append(m)
    res = run_bass_kernel_spmd(nc, in_maps, core_ids=list(range(8)))
    out = np.stack([res.results[b]["out"] for b in range(B)], axis=0)
    return out.astype(np.float32)
```
